# Optimizing a Trainium2 kernel written in Bass

```python
import math
import jax, jax.numpy as jnp
from jax import lax
import numpy as np

D_MODEL = 1024
BATCH = 8
SEQ = 4096
DEPTH = 2

HEAD_DIM = 64
N_HEADS_SB = 8
N_HEADS_FOX = 8
W_SB = N_HEADS_SB * HEAD_DIM
W_FOX = N_HEADS_FOX * HEAD_DIM
Q_BLOCK = 128
D_FF = 2816
N_EXPERTS = 8
TOP_K = 2
D_FF_EXPERT = 1408
N_DENSE = (DEPTH + 1) // 2
N_MOE = DEPTH // 2
RMS_EPS = 1e-6
PROJ_IN = 3 * W_SB + 3 * W_FOX + N_HEADS_FOX + 2 * D_MODEL

kernel_name = "hybrid_stickbreaking_fox_gated_moe"


def rmsnorm(x, g):
    x32 = x.astype(jnp.float32)
    y = x32 * lax.rsqrt(jnp.mean(x32 * x32, axis=-1, keepdims=True) + RMS_EPS)
    return (y * g.astype(jnp.float32)).astype(x.dtype)


def split_heads(t, n_heads):
    b, s, _ = t.shape
    return t.reshape(b, s, n_heads, HEAD_DIM).transpose(0, 2, 1, 3)


def merge_heads(t):
    b, h, s, d = t.shape
    return t.transpose(0, 2, 1, 3).reshape(b, s, h * d)


def stick_breaking_attention(q, k, v):
    seq = q.shape[2]
    scale = 1.0 / math.sqrt(HEAD_DIM)
    outs = []
    for i in range(seq // Q_BLOCK):
        t0 = i * Q_BLOCK
        kv_len = t0 + Q_BLOCK
        qb = q[:, :, t0:kv_len]
        kb = k[:, :, :kv_len]
        vb = v[:, :, :kv_len]
        z = jnp.einsum('bhqd,bhkd->bhqk', qb, kb).astype(jnp.float32) * scale
        q_pos = t0 + jnp.arange(Q_BLOCK)[:, None]
        k_pos = jnp.arange(kv_len)[None, :]
        mask = k_pos < q_pos
        log_beta = jax.nn.log_sigmoid(z)
        log_keep = jnp.where(mask, jax.nn.log_sigmoid(-z), 0.0)
        tail = lax.cumsum(log_keep, axis=log_keep.ndim - 1, reverse=True) - log_keep
        w = jnp.where(mask, jnp.exp(log_beta + tail), 0.0)
        outs.append(jnp.einsum('bhqk,bhkd->bhqd', w.astype(vb.dtype), vb))
    return jnp.concatenate(outs, axis=2)


def forgetting_attention(q, k, v, log_f):
    seq = q.shape[2]
    scale = 1.0 / math.sqrt(HEAD_DIM)
    cum_f = lax.cumsum(log_f, axis=log_f.ndim - 1)
    outs = []
    for i in range(seq // Q_BLOCK):
        t0 = i * Q_BLOCK
        kv_len = t0 + Q_BLOCK
        qb = q[:, :, t0:kv_len]
        kb = k[:, :, :kv_len]
        vb = v[:, :, :kv_len]
        z = jnp.einsum('bhqd,bhkd->bhqk', qb, kb).astype(jnp.float32) * scale
        decay = cum_f[:, :, t0:kv_len, None] - cum_f[:, :, None, :kv_len]
        q_pos = t0 + jnp.arange(Q_BLOCK)[:, None]
        k_pos = jnp.arange(kv_len)[None, :]
        logits = jnp.where(k_pos <= q_pos, z + decay, -jnp.inf)
        p = jax.nn.softmax(logits, axis=-1)
        outs.append(jnp.einsum('bhqk,bhkd->bhqd', p.astype(vb.dtype), vb))
    return jnp.concatenate(outs, axis=2)


def swiglu(h, w_gu, w_dn):
    gu = h @ w_gu
    g, u = jnp.split(gu, 2, axis=-1)
    return (jax.nn.silu(g) * u) @ w_dn


def moe_swiglu(h, w_router, w_gu_e, w_dn_e):
    logits = (h @ w_router).astype(jnp.float32)
    top_val, top_idx = lax.top_k(logits, TOP_K)
    top_w = jax.nn.softmax(top_val, axis=-1)
    combine = jnp.sum(jax.nn.one_hot(top_idx, N_EXPERTS, dtype=jnp.float32) * top_w[..., None], axis=-2)
    combine = combine.astype(h.dtype)
    out = jnp.zeros_like(h)
    for e in range(N_EXPERTS):
        out = out + combine[..., e:e + 1] * swiglu(h, w_gu_e[e], w_dn_e[e])
    return out


def setup_inputs(seed: int = 0) -> dict:
    key = jax.random.key(seed)
    ks = jax.random.split(key, 16)
    nrm = lambda k, shape, fan_in: jax.random.normal(k, shape, jnp.float32) * fan_in ** -0.5
    return {
        "x": jax.random.normal(ks[0], (BATCH, SEQ, D_MODEL), jnp.float32),
        "g_mix": 1.0 + 0.02 * jax.random.normal(ks[1], (DEPTH, D_MODEL), jnp.float32),
        "w_in": nrm(ks[2], (DEPTH, D_MODEL, PROJ_IN), D_MODEL),
        "b_f": 2.0 + 0.1 * jax.random.normal(ks[3], (DEPTH, N_HEADS_FOX), jnp.float32),
        "b_gate": 0.02 * jax.random.normal(ks[4], (DEPTH, 2 * D_MODEL), jnp.float32),
        "g_q": 1.0 + 0.02 * jax.random.normal(ks[5], (DEPTH, HEAD_DIM), jnp.float32),
        "g_k": 1.0 + 0.02 * jax.random.normal(ks[6], (DEPTH, HEAD_DIM), jnp.float32),
        "w_o_sb": nrm(ks[7], (DEPTH, W_SB, D_MODEL), W_SB),
        "w_o_fox": nrm(ks[8], (DEPTH, W_FOX, D_MODEL), W_FOX),
        "w_out": nrm(ks[9], (DEPTH, D_MODEL, D_MODEL), D_MODEL),
        "g_ffn": 1.0 + 0.02 * jax.random.normal(ks[10], (DEPTH, D_MODEL), jnp.float32),
        "w_gu_dense": nrm(ks[11], (N_DENSE, D_MODEL, 2 * D_FF), D_MODEL),
        "w_dn_dense": nrm(ks[12], (N_DENSE, D_FF, D_MODEL), D_FF),
        "w_router": nrm(ks[13], (N_MOE, D_MODEL, N_EXPERTS), D_MODEL),
        "w_gu_exp": nrm(ks[14], (N_MOE, N_EXPERTS, D_MODEL, 2 * D_FF_EXPERT), D_MODEL),
        "w_dn_exp": nrm(ks[15], (N_MOE, N_EXPERTS, D_FF_EXPERT, D_MODEL), D_FF_EXPERT),
    }


def reference(x, g_mix, w_in, b_f, b_gate, g_q, g_k, w_o_sb, w_o_fox, w_out,
              g_ffn, w_gu_dense, w_dn_dense, w_router, w_gu_exp, w_dn_exp):
    splits = [int(c) for c in np.cumsum([W_SB, W_SB, W_SB, W_FOX, W_FOX, W_FOX, N_HEADS_FOX])]
    for l in range(DEPTH):
        h = rmsnorm(x, g_mix[l])
        p = h @ w_in[l]
        q_sb, k_sb, v_sb, q_fx, k_fx, v_fx, f_pre, gate_pre = jnp.split(p, splits, axis=-1)

        y_sb = stick_breaking_attention(split_heads(q_sb, N_HEADS_SB),
                                        split_heads(k_sb, N_HEADS_SB),
                                        split_heads(v_sb, N_HEADS_SB))
        y_sb = merge_heads(y_sb) @ w_o_sb[l]

        qf = rmsnorm(split_heads(q_fx, N_HEADS_FOX), g_q[l])
        kf = rmsnorm(split_heads(k_fx, N_HEADS_FOX), g_k[l])
        log_f = jax.nn.log_sigmoid(f_pre.astype(jnp.float32) + b_f[l].astype(jnp.float32))
        y_fx = forgetting_attention(qf, kf, split_heads(v_fx, N_HEADS_FOX), log_f.transpose(0, 2, 1))
        y_fx = merge_heads(y_fx) @ w_o_fox[l]

        g_sb, g_fx = jnp.split(jax.nn.sigmoid(gate_pre + b_gate[l]), 2, axis=-1)
        x = x + (g_sb * y_sb + g_fx * y_fx) @ w_out[l]

        h = rmsnorm(x, g_ffn[l])
        if l % 2 == 0:
            x = x + swiglu(h, w_gu_dense[l // 2], w_dn_dense[l // 2])
        else:
            x = x + moe_swiglu(h, w_router[l // 2], w_gu_exp[l // 2], w_dn_exp[l // 2])
    return x
```

```python
from contextlib import ExitStack
import numpy as np
import concourse.bass as bass
import concourse.mybir as mybir
from concourse.bass_utils import run_bass_kernel_spmd

F32 = mybir.dt.float32
BF16 = mybir.dt.bfloat16
I32 = mybir.dt.int32
AF = mybir.ActivationFunctionType
ALU = mybir.AluOpType
AX = mybir.AxisListType

D = 1024
KC = 8
HD = 64
NH = 8
PROJ_IN = 5128
DFF = 2816
NE = 8
FE = 1408
FJ = 11
EPS = 1e-6
CH = 512

SEM_LIMIT = 20000
N_DMA_SEMS = 24


class Op:
    __slots__ = ("eng", "fn", "deps", "dma", "milestone", "sem", "count", "dsem", "dcount", "dprev")

    def __init__(self, eng, fn, deps, dma):
        self.eng = eng
        self.fn = fn
        self.deps = deps
        self.dma = dma
        self.milestone = False
        self.sem = None
        self.count = 0
        self.dsem = None
        self.dcount = 0
        self.dprev = 0


class Sched:
    ENGS = ("pe", "act", "dve", "pool", "sp")

    def __init__(self, nc, same_engine_sync=True):
        self.nc = nc
        self.ops = []
        self.lastw = {}
        self.rd = {}
        self.same = same_engine_sync
        self.last_on = {e: None for e in self.ENGS}
        self.pending_barrier = {e: None for e in self.ENGS}
        self.dma_since = []

    def add(self, eng, fn, reads=(), writes=(), dma=False):
        idx = len(self.ops)
        deps = set()
        for k in reads:
            w = self.lastw.get(k)
            if w is not None:
                deps.add(w)
        for k in writes:
            w = self.lastw.get(k)
            if w is not None:
                deps.add(w)
            for r in self.rd.get(k, ()):
                deps.add(r)
        for k in reads:
            lst = self.rd.setdefault(k, [])
            if not dma:
                lst[:] = [r for r in lst if self.ops[r].dma or self.ops[r].eng != eng]
            lst.append(idx)
        for k in writes:
            self.lastw[k] = idx
            self.rd[k] = []
        pb = self.pending_barrier[eng]
        if pb is not None:
            deps |= pb
            self.pending_barrier[eng] = None
        deps.discard(idx)
        self.ops.append(Op(eng, fn, deps, dma))
        self.last_on[eng] = idx
        if dma:
            self.dma_since.append(idx)
        return idx

    def barrier(self):
        deps = set(i for i in self.last_on.values() if i is not None)
        deps |= set(self.dma_since)
        self.dma_since = []
        for e in self.ENGS:
            cur = self.pending_barrier[e]
            self.pending_barrier[e] = (cur | deps) if cur is not None else set(deps)

    def finish(self):
        self.barrier()
        self.add("sp", None)

    def emit(self):
        nc = self.nc
        ops = self.ops
        for op in ops:
            for d in op.deps:
                p = ops[d]
                if p.dma:
                    continue
                if p.eng != op.eng:
                    p.milestone = True
                elif self.same and op.eng in ("act", "dve", "pool"):
                    p.milestone = True
        n_epochs = {e: 1 for e in self.ENGS}
        cnt = {e: 0 for e in self.ENGS}
        ep = {e: 0 for e in self.ENGS}
        dma_uses = [0] * N_DMA_SEMS
        dma_rr = 0
        for op in ops:
            if op.dma:
                k = dma_rr % N_DMA_SEMS
                dma_rr += 1
                op.dsem = k
                op.dprev = 16 * dma_uses[k]
                dma_uses[k] += 1
                op.dcount = 16 * dma_uses[k]
            elif op.milestone:
                e = op.eng
                if cnt[e] >= SEM_LIMIT:
                    ep[e] += 1
                    cnt[e] = 0
                    n_epochs[e] = ep[e] + 1
                cnt[e] += 1
                op.sem = (e, ep[e])
                op.count = cnt[e]
        with ExitStack() as st:
            sems = {}
            for e in self.ENGS:
                for k in range(n_epochs[e]):
                    sems[(e, k)] = st.enter_context(nc.semaphore(f"s_{e}_{k}"))
            dsems = [st.enter_context(nc.semaphore(f"s_dma_{k}")) for k in range(N_DMA_SEMS)]
            block = st.enter_context(nc.Block())

            def run_engine(eng_name, handle):
                waited = {}
                for op in ops:
                    if op.eng != eng_name:
                        continue
                    need = {}
                    for d in op.deps:
                        p = ops[d]
                        if p.dma:
                            key = ("d", p.dsem)
                            val = p.dcount
                        else:
                            if not p.milestone:
                                continue
                            if p.eng == eng_name and not (self.same and eng_name in ("act", "dve", "pool")):
                                continue
                            key = p.sem
                            val = p.count
                        if need.get(key, 0) < val:
                            need[key] = val
                    if op.dma and op.dprev > 0:
                        key = ("d", op.dsem)
                        if need.get(key, 0) < op.dprev:
                            need[key] = op.dprev
                    for key, val in need.items():
                        if key[0] == "d":
                            if waited.get(key, 0) >= val:
                                continue
                            handle.wait_ge(dsems[key[1]], val)
                            waited[key] = val
                        else:
                            e, k = key
                            w = waited.get(e, (-1, 0))
                            if w[0] > k or (w[0] == k and w[1] >= val):
                                continue
                            handle.wait_ge(sems[key], val)
                            waited[e] = (k, val)
                    if op.fn is None:
                        continue
                    ins = op.fn(handle)
                    if op.dma:
                        ins.then_inc(dsems[op.dsem], 16)
                    elif op.milestone:
                        ins.then_inc(sems[op.sem], 1)

            @block.tensor
            def _(h):
                run_engine("pe", h)

            @block.scalar
            def _(h):
                run_engine("act", h)

            @block.vector
            def _(h):
                run_engine("dve", h)

            @block.gpsimd
            def _(h):
                run_engine("pool", h)

            @block.sync
            def _(h):
                run_engine("sp", h)


class Arena:
    def __init__(self, ar, words):
        self.ar = ar
        self.words = words
        self.top = 0

    def mark(self):
        return self.top

    def release(self, m):
        self.top = m

    def f32(self, n, parts=128):
        o = self.top
        self.top += n
        assert self.top <= self.words, f"arena overflow {self.top}"
        return self.ar[0:parts, o:o + n]

    def bf(self, n, parts=128):
        w = (n + 1) // 2
        o = self.top
        self.top += w
        assert self.top <= self.words, f"arena overflow {self.top}"
        return self.ar[0:parts, o:o + w].bitcast(BF16)[:, 0:n]

    def i32(self, n):
        o = self.top
        self.top += n
        assert self.top <= self.words
        return self.ar[:, o:o + n].bitcast(I32)


def build_nc(S, n_layers=2, same_engine_sync=True):
    NCH = S // CH
    NB = S // 128
    SC = min(2048, S)
    NSC = S // SC
    CPS = SC // CH
    BPS = SC // 128

    nc = bass.Bass("TRN2", target_bir_lowering=False)
    dr = {}

    def din(name, shape, dt=F32):
        dr[name] = nc.dram_tensor(name, list(shape), dt, kind="ExternalInput").ap()
        return dr[name]

    xT_in = din("xT", [D, S])
    prm_in = din("prm", [2, 128, 64])
    w_in = din("w_in", [2, D, PROJ_IN])
    w_o_sb = din("w_o_sb", [2, 512, D])
    w_o_fox = din("w_o_fox", [2, 512, D])
    w_out = din("w_out", [2, D, D])
    w_gu_dense = din("w_gu_dense", [1, D, 2 * DFF])
    w_dn_dense = din("w_dn_dense", [1, DFF, D])
    w_router = din("w_router", [1, D, NE])
    w_gu_exp = din("w_gu_exp", [1, NE, D, 2 * FE])
    w_dn_exp = din("w_dn_exp", [1, NE, FE, D])
    outT = nc.dram_tensor("outT", [D, S], F32, kind="ExternalOutput").ap()
    x1T = nc.dram_tensor("x1T_scr", [D, S], F32, kind="Internal").ap()
    xmidT = nc.dram_tensor("xmidT_scr", [D, S], F32, kind="Internal").ap()
    ysb_scr = nc.dram_tensor("ysb_scr", [4, 128, S], BF16, kind="Internal").ap()
    yfx_scr = nc.dram_tensor("yfx_scr", [4, 128, S], BF16, kind="Internal").ap()

    WORDS = 53000
    with ExitStack() as st:
        ar_t = st.enter_context(nc.sbuf_tensor("arena", [128, WORDS], F32))
        psall = st.enter_context(nc.psum_tensor("psall", [128, 8 * 512], F32))[:, :]
        ps = [psall[:, b * 512:(b + 1) * 512] for b in range(8)]

        def ps2(b):
            return psall[:, b * 512:(b + 2) * 512].rearrange("p (i n) -> p i n", i=2)
        A = Arena(ar_t, WORDS)
        Sx = Sched(nc, same_engine_sync=same_engine_sync)
        add = Sx.add

        def mm(out, lhsT, rhs, start, stop, reads, writes, sgc=False):
            if sgc:
                add("pe", lambda h: h.matmul(out, lhsT, rhs, start=start, stop=stop, skip_group_check=True), reads, writes)
            else:
                add("pe", lambda h: h.matmul(out, lhsT, rhs, start=start, stop=stop), reads, writes)

        def act(out, in_, func, reads, writes, scale=1.0, bias=None):
            if bias is None:
                add("act", lambda h: h.activation(out=out, in_=in_, func=func, scale=scale), reads, writes)
            else:
                add("act", lambda h: h.activation(out=out, in_=in_, func=func, scale=scale, bias=bias), reads, writes)

        def tt(out, in0, in1, op, reads, writes, eng="dve"):
            add(eng, lambda h: h.tensor_tensor(out=out, in0=in0, in1=in1, op=op), reads, writes)

        def ts(out, in0, s1, op0, reads, writes, s2=None, op1=None, eng="dve"):
            if op1 is None:
                add(eng, lambda h: h.tensor_scalar(out=out, in0=in0, scalar1=s1, scalar2=None, op0=op0), reads, writes)
            else:
                add(eng, lambda h: h.tensor_scalar(out=out, in0=in0, scalar1=s1, scalar2=s2, op0=op0, op1=op1), reads, writes)

        def stt(out, in0, scalar, in1, op0, op1, reads, writes):
            add("dve", lambda h: h.scalar_tensor_tensor(out=out, in0=in0, scalar=scalar, in1=in1, op0=op0, op1=op1), reads, writes)

        def cp(out, in_, reads, writes, eng="dve"):
            add(eng, lambda h: h.tensor_copy(out=out, in_=in_), reads, writes)

        def dma(q, out, in_, reads, writes):
            add(q, lambda h: h.dma_start(out=out, in_=in_), reads, writes, dma=True)

        stg = {"bufs": [], "n": 0}

        def stg_alloc(words, n):
            stg["bufs"] = [A.f32(words) for _ in range(n)]

        def wload(dst, src, wkey, a, b, parts=128):
            k = stg["n"] % len(stg["bufs"])
            stg["n"] += 1
            sv = stg["bufs"][k][0:parts, 0:a * b].rearrange("p (a b) -> p a b", a=a)
            dma("sp", sv, src, (), (("stg", k),))
            cp(dst, sv, (("stg", k),), (wkey,), eng="pool")

        ones_bf = A.bf(128)
        negtri_bf = A.bf(128)
        bdiag_bf = A.bf(128)
        ident_f = A.f32(128)
        ones_f = A.f32(128)
        trile_f = A.f32(128)
        maskSB = [A.bf(512) for _ in range(4)]
        maskFX = [A.bf(512) for _ in range(4)]
        prm = [A.f32(64) for _ in range(2)]
        io_t = A.i32(512)

        add("dve", lambda h: h.memset(ones_bf, 1.0), (), ("c_ones",))
        add("dve", lambda h: h.memset(ones_f, 1.0), (), ("c_onesf",))
        add("dve", lambda h: h.memset(bdiag_bf, 0.0), (), ("c_bdiag",))
        add("dve", lambda h: h.memset(bdiag_bf[0:64, 0:64], 1.0), (), ("c_bdiag",))
        add("dve", lambda h: h.memset(bdiag_bf[64:128, 64:128], 1.0), (), ("c_bdiag",))
        add("pool", lambda h: h.iota(io_t[:, 0:128], pattern=[[-1, 128]], base=0, channel_multiplier=1), (), ("iota",))
        ts(negtri_bf, io_t[:, 0:128], 0.0, ALU.is_ge, ("iota",), ("c_negtri",), s2=-1.0, op1=ALU.mult)
        ts(trile_f, io_t[:, 0:128], 0.0, ALU.is_le, ("iota",), ("c_trile",))
        ts(ident_f, io_t[:, 0:128], 0.0, ALU.is_equal, ("iota",), ("c_ident",))
        for j in range(4):
            add("pool", lambda h, j=j: h.iota(io_t[:, 0:512], pattern=[[1, 512]], base=-128 * j, channel_multiplier=-1),
                ("iota",), ("iota",))
            ts(maskSB[j], io_t[:, 0:512], 0.0, ALU.is_gt, ("iota",), ("c_mask",))
            ts(maskFX[j], io_t[:, 0:512], 0.0, ALU.is_ge, ("iota",), ("c_mask",))
        for l in range(2):
            dma("sp", prm[l], prm_in[l], (), ("prm", ))
        CONST_TOP = A.mark()

        def chunk_src(src, c):
            return src[:, c * CH:(c + 1) * CH].rearrange("(k p) n -> p k n", p=128)

        def rms_chunk(xb, xkey, sq, gcol0, prm_l, out_fn, tagp, want_f32=None):
            lnv = tagp["lnv"]
            rstd = tagp["rstd"]
            act(sq, xb, AF.Square, (xkey,), ("sq",))
            for kc in range(KC):
                mm(ps[7], ones_bf, sq[:, kc, :], kc == 0, kc == KC - 1, ("sq", "c_ones"), (("ps", 7),))
            act(lnv, ps[7], AF.Ln, (("ps", 7),), ("lnv",), scale=1.0 / D, bias=EPS)
            act(rstd, lnv, AF.Exp, ("lnv",), ("rstd",), scale=-0.5)
            for kc in range(KC):
                o, okey = out_fn(kc)
                stt(o, xb[:, kc, :], prm_l[:, gcol0 + kc:gcol0 + kc + 1], rstd, ALU.mult, ALU.mult,
                    (xkey, "rstd", "prm"), (okey,))
                if want_f32 is not None:
                    o2, o2key = want_f32(kc)
                    stt(o2, xb[:, kc, :], prm_l[:, gcol0 + kc:gcol0 + kc + 1], rstd, ALU.mult, ALU.mult,
                        (xkey, "rstd", "prm"), (o2key,))

        for l in range(n_layers):
            prm_l = prm[l]
            w_in_l = w_in[l]
            x_src = xT_in if l == 0 else xmidT
            x_dst = outT if l == n_layers - 1 else xmidT
            A.release(CONST_TOP)
            Sx.barrier()
            hT = A.bf(KC * S).rearrange("p (k n) -> p k n", k=KC)
            MIX_TOP = A.mark()
            xb = [A.f32(KC * CH).rearrange("p (k n) -> p k n", k=KC) for _ in range(2)]
            sq = A.bf(KC * CH).rearrange("p (k n) -> p k n", k=KC)
            tagp = {"lnv": A.f32(CH), "rstd": A.f32(CH)}
            dma("sp", xb[0], chunk_src(x_src, 0), (), (("xb", 0),))
            for c in range(NCH):
                if c + 1 < NCH:
                    dma("sp", xb[(c + 1) % 2], chunk_src(x_src, c + 1), (), (("xb", (c + 1) % 2),))
                rms_chunk(xb[c % 2], ("xb", c % 2), sq, 0, prm_l,
                          lambda kc, c=c: (hT[:, kc, c * CH:(c + 1) * CH], ("hT", c)), tagp)
            A.release(MIX_TOP)
            Sx.barrier()

            V = A.bf(NB * 512).rearrange("p (b n) -> p b n", b=NB)
            regA = A.f32(2 * 2 * CH)
            regB = A.f32(2 * 2 * CH)
            wv = regA.bitcast(BF16).rearrange("p (k n) -> p k n", k=KC)
            qTh = [A.bf(S), A.bf(S)]
            kT = A.bf(S)
            wq = [A.bf(KC * 128).rearrange("p (k n) -> p k n", k=KC) for _ in range(2)]
            wk = [A.bf(KC * 128).rearrange("p (k n) -> p k n", k=KC) for _ in range(2)]
            e2 = [regA[:, k_ * 2 * CH:(k_ + 1) * 2 * CH].rearrange("p (i n) -> p i n", i=2) for k_ in range(2)]
            L2 = [A.bf(2 * CH).rearrange("p (i n) -> p i n", i=2) for _ in range(3)]
            w2 = [A.bf(2 * CH).rearrange("p (i n) -> p i n", i=2) for _ in range(3)]
            tmp2 = [regB[:, k_ * 2 * CH:(k_ + 1) * 2 * CH].rearrange("p (i n) -> p i n", i=2) for k_ in range(2)]
            R2 = A.f32(2 * CH).rearrange("p (i n) -> p i n", i=2)
            w_sb = [A.bf(CH) for _ in range(4)]
            yo = [A.bf(CH) for _ in range(2)]
            rec = [A.f32(CH) for _ in range(2)]
            qraw = A.f32(CH)
            sq2 = A.bf(CH)
            lnv2 = A.f32(CH)
            r2 = A.f32(CH)
            wf = A.bf(KC * 8).rearrange("p (k n) -> p k n", k=KC)
            fpre = A.f32(NB * 8)
            fe = A.f32(NB * 8)
            fl = A.f32(NB * 8)
            tot = A.f32(NB * 8)
            G = A.f32(NB * 8)
            off = A.f32((NB + 1) * 8)
            assert NCH * NH * NB <= 2 * 2 * CH
            biasT = [regB[:, c_ * NH * NB:(c_ + 1) * NH * NB].rearrange("p (h b) -> p h b", h=NH) for c_ in range(NCH)]
            cnt = {"P": 0}
            stg_alloc(KC * 128, 2)

            add("dve", lambda h_: h_.memset(qTh[0][64:128, :], 0.0), (), ("qz0",))
            add("dve", lambda h_: h_.memset(qTh[1][0:64, :], 0.0), (), ("qz1",))

            def proj_V(col0):
                for q4 in range(4):
                    wload(wv[:, :, q4 * 128:(q4 + 1) * 128],
                          w_in_l[:, col0 + q4 * 128:col0 + (q4 + 1) * 128].rearrange("(k p) n -> p k n", p=128), "wv", KC, 128)
                for b in range(NB):
                    pbi = b % 4
                    pb = ps[pbi]
                    for kc in range(KC):
                        mm(pb, hT[:, kc, b * 128:(b + 1) * 128], wv[:, kc, :], kc == 0, kc == KC - 1,
                           (("hT", b // 4), "wv"), (("ps", pbi),))
                    if b % 2 == 0:
                        act(V[:, b, :], pb, AF.Copy, (("ps", pbi),), (("V", b),))
                    else:
                        cp(V[:, b, :], pb, (("ps", pbi),), (("V", b),))

            def load_qk_w(g, qcol0, kcol0):
                par = g % 2
                wload(wq[par], w_in_l[:, qcol0 + g * 128:qcol0 + (g + 1) * 128].rearrange("(k p) n -> p k n", p=128),
                      ("wq", par), KC, 128)
                wload(wk[par], w_in_l[:, kcol0 + g * 128:kcol0 + (g + 1) * 128].rearrange("(k p) n -> p k n", p=128),
                      ("wk", par), KC, 128)

            def proj_qk_sb(g):
                par = g % 2
                for c in range(NCH):
                    cs = slice(c * CH, (c + 1) * CH)
                    pb = cnt["P"] % 4
                    cnt["P"] += 1
                    for kc in range(KC):
                        mm(ps[pb], wq[par][:, kc, :], hT[:, kc, cs], kc == 0, kc == KC - 1, (("hT", c), ("wq", par)), (("ps", pb),))
                    act(qTh[0][0:64, cs], ps[pb][0:64, :], AF.Copy, (("ps", pb), "qz0"), (("qT", 0, c),), scale=0.125)
                    act(qTh[1][64:128, cs], ps[pb][64:128, :], AF.Copy, (("ps", pb), "qz1"), (("qT", 1, c),), scale=0.125)
                    pb = cnt["P"] % 4
                    cnt["P"] += 1
                    for kc in range(KC):
                        mm(ps[pb], wk[par][:, kc, :], hT[:, kc, cs], kc == 0, kc == KC - 1, (("hT", c), ("wk", par)), (("ps", pb),))
                    cp(kT[:, cs], ps[pb], (("ps", pb),), (("kT", c),))

            def proj_qk_fx(g):
                par = g % 2
                for c in range(NCH):
                    cs = slice(c * CH, (c + 1) * CH)
                    for isq in (True, False):
                        wt, wkey = (wq[par], ("wq", par)) if isq else (wk[par], ("wk", par))
                        pb = cnt["P"] % 4
                        cnt["P"] += 1
                        for kc in range(KC):
                            mm(ps[pb], wt[:, kc, :], hT[:, kc, cs], kc == 0, kc == KC - 1, (("hT", c), wkey), (("ps", pb),))
                        cp(qraw, ps[pb], (("ps", pb),), ("qraw",))
                        act(sq2, qraw, AF.Square, ("qraw",), ("sq2",))
                        mm(ps[6], bdiag_bf, sq2, True, True, ("sq2", "c_bdiag"), (("ps", 6),))
                        act(lnv2, ps[6], AF.Ln, (("ps", 6),), ("lnv2",), scale=1.0 / HD, bias=EPS)
                        if isq:
                            act(r2, lnv2, AF.Exp, ("lnv2",), ("r2",), scale=-0.5, bias=-2.0794415416798357)
                            stt(qTh[0][0:64, cs], qraw[0:64, :], prm_l[0:64, 32:33], r2[0:64, :], ALU.mult, ALU.mult,
                                ("qraw", "r2", "prm", "qz0"), (("qT", 0, c),))
                            stt(qTh[1][64:128, cs], qraw[64:128, :], prm_l[64:128, 32:33], r2[64:128, :], ALU.mult, ALU.mult,
                                ("qraw", "r2", "prm", "qz1"), (("qT", 1, c),))
                        else:
                            act(r2, lnv2, AF.Exp, ("lnv2",), ("r2",), scale=-0.5)
                            stt(kT[:, cs], qraw, prm_l[:, 33:34], r2, ALU.mult, ALU.mult, ("qraw", "r2", "prm"), (("kT", c),))

            def tasks_for(desc):
                tl = []
                for c in range(NCH):
                    nkb = 4 * (c + 1)
                    kbs = list(range(nkb))
                    if desc:
                        kbs = kbs[::-1]
                    for n_, kb in enumerate(kbs):
                        for i in (0, 1):
                            tl.append((c, kb, i, n_ == 0, n_ == nkb - 1))
                return tl

            def attn_sb_pair(g):
                tl = []
                for c in range(NCH):
                    nkb = 4 * (c + 1)
                    for n_, kb in enumerate(reversed(range(nkb))):
                        tl.append((c, kb, n_ == 0, n_ == nkb - 1))
                N = len(tl)
                vs = slice(g * 128, (g + 1) * 128)

                def info(n):
                    c, kb, first, last = tl[n]
                    j = kb - 4 * c
                    c0 = 128 * j if j > 0 else 0
                    return c, kb, first, last, j, c0

                def s1(n):
                    c, kb, first, last, j, c0 = info(n)
                    sl = n % 2
                    Aks = (("ps", 2 * sl), ("ps", 2 * sl + 1))
                    for i in (0, 1):
                        mm(ps[2 * sl + i][:, c0:], kT[:, kb * 128:(kb + 1) * 128], qTh[i][:, c * CH + c0:(c + 1) * CH], True, True,
                           (("kT", kb // 4), ("qT", i, c)), (Aks[i],))
                    act(e2[sl][:, :, c0:], ps2(2 * sl)[:, :, c0:], AF.Exp, Aks, (("e2", sl),))
                    act(L2[n % 3][:, :, c0:], e2[sl][:, :, c0:], AF.Ln, (("e2", sl),), (("L2", n % 3),), bias=1.0)
                    if j >= 0:
                        for i in (0, 1):
                            tt(L2[n % 3][:, i, c0:], L2[n % 3][:, i, c0:], maskSB[j][:, c0:], ALU.mult,
                               (("L2", n % 3), "c_mask"), (("L2", n % 3),), eng="pool")

                def s2(n):
                    c, kb, first, last, j, c0 = info(n)
                    sl = n % 2
                    Aks = (("ps", 2 * sl), ("ps", 2 * sl + 1))
                    Lk = ("L2", n % 3)
                    for i in (0, 1):
                        mm(ps[2 * sl + i][:, c0:], negtri_bf, L2[n % 3][:, i, c0:], False, True, (Lk, "c_negtri"), (Aks[i],), sgc=True)
                    if not last:
                        for i in (0, 1):
                            mm(ps[6 + i][:, c0:], ones_bf, L2[n % 3][:, i, c0:], True, True, (Lk, "c_ones"), (("ps", 6 + i),))
                    wk_ = ("w2", n % 3)
                    if first:
                        act(w2[n % 3][:, :, c0:], ps2(2 * sl)[:, :, c0:], AF.Exp, Aks, (wk_,))
                    else:
                        tt(tmp2[sl][:, :, c0:], ps2(2 * sl)[:, :, c0:], R2[:, :, c0:], ALU.subtract, Aks + ("R2",), (("tmp2", sl),))
                        act(w2[n % 3][:, :, c0:], tmp2[sl][:, :, c0:], AF.Exp, (("tmp2", sl),), (wk_,))
                    if j >= 0:
                        for i in (0, 1):
                            tt(w2[n % 3][:, i, c0:], w2[n % 3][:, i, c0:], maskSB[j][:, c0:], ALU.mult, (wk_, "c_mask"), (wk_,), eng="pool")
                    if not last:
                        Bks = (("ps", 6), ("ps", 7))
                        if first:
                            if c0 > 0:
                                add("dve", lambda h_: h_.memset(R2[:, :, 0:c0], 0.0), (), ("R2",))
                            cp(R2[:, :, c0:], ps2(6)[:, :, c0:], Bks, ("R2",))
                        else:
                            tt(R2[:, :, c0:], ps2(6)[:, :, c0:], R2[:, :, c0:], ALU.add, Bks + ("R2",), ("R2",))

                def s3(n):
                    c, kb, first, last, j, c0 = info(n)
                    for i in (0, 1):
                        ob = 4 + i
                        mm(ps[ob][:, c0:], V[:, kb, vs], w2[n % 3][:, i, c0:], first, last, (("w2", n % 3), ("V", kb)), (("ps", ob),))
                    if last:
                        yb = c % 2
                        cp(yo[yb][0:64, :], ps[4][0:64, :], (("ps", 4),), (("yo", yb, 0),))
                        act(yo[yb][64:128, :], ps[5][64:128, :], AF.Copy, (("ps", 5),), (("yo", yb, 1),))
                        dma("sp", ysb_scr[g, :, c * CH:(c + 1) * CH], yo[yb], (("yo", yb, 0), ("yo", yb, 1)), ())

                for it in range(N + 2):
                    if it < N:
                        s1(it)
                    if 0 <= it - 1 < N:
                        s2(it - 1)
                    if 0 <= it - 2 < N:
                        s3(it - 2)

            def attn_fx_pair(g):
                tl = tasks_for(False)
                N = len(tl)
                vs = slice(g * 128, (g + 1) * 128)

                def s1(n):
                    c, kb, i, first, last = tl[n]
                    ab = n % 4
                    Ak = ("ps", ab)
                    j = kb - 4 * c
                    h = 2 * g + i
                    c0 = 128 * j if j > 0 else 0
                    mm(ps[ab][:, c0:], kT[:, kb * 128:(kb + 1) * 128], qTh[i][:, c * CH + c0:(c + 1) * CH], True, True,
                       (("kT", kb // 4), ("qT", i, c)), (Ak,))
                    act(w_sb[ab][:, c0:], ps[ab][:, c0:], AF.Exp, (Ak, "biasT"), (("w", ab),), bias=biasT[c][:, h, kb:kb + 1])
                    if j >= 0:
                        tt(w_sb[ab][:, c0:], w_sb[ab][:, c0:], maskFX[j][:, c0:], ALU.mult, (("w", ab), "c_mask"), (("w", ab),), eng="pool")

                def s2(n):
                    c, kb, i, first, last = tl[n]
                    ab = n % 4
                    ob = 4 + i
                    db = 6 + i
                    j = kb - 4 * c
                    c0 = 128 * j if j > 0 else 0
                    mm(ps[ob][:, c0:], V[:, kb, vs], w_sb[ab][:, c0:], first, last, (("w", ab), ("V", kb)), (("ps", ob),))
                    mm(ps[db][:, c0:], ones_bf, w_sb[ab][:, c0:], first, last, (("w", ab), "c_ones"), (("ps", db),))
                    if last:
                        yb = c % 2
                        r = slice(64 * i, 64 * i + 64)
                        recf = rec[0]
                        act(yo[yb][r, :], ps[ob][r, :], AF.Copy, (("ps", ob),), (("yo", yb, i),))
                        cp(recf[r, :], ps[db][r, :], (("ps", db),), (("rec", i),))
                        if i == 1:
                            rk = (("rec", 0), ("rec", 1))
                            yk = (("yo", yb, 0), ("yo", yb, 1))
                            add("dve", lambda h_: h_.reciprocal(out=recf, in_=recf), rk, rk)
                            tt(yo[yb], yo[yb], recf, ALU.mult, yk + rk, yk)
                            dma("sp", yfx_scr[g, :, c * CH:(c + 1) * CH], yo[yb], yk, ())

                for it in range(N + 3):
                    if it < N:
                        s1(it)
                    if 0 <= it - 3 < N:
                        s2(it - 3)

            proj_V(1024)
            Sx.barrier()
            load_qk_w(0, 0, 512)
            for g in range(4):
                if g + 1 < 4:
                    load_qk_w(g + 1, 0, 512)
                proj_qk_sb(g)
                attn_sb_pair(g)
            Sx.barrier()
            proj_V(2560)
            Sx.barrier()
            wload(wf, w_in_l[:, 3072:3080].rearrange("(k p) n -> p k n", p=128), "wf", KC, 8)
            for b in range(NB):
                for kc in range(KC):
                    mm(ps[7][:, b * 8:(b + 1) * 8], hT[:, kc, b * 128:(b + 1) * 128], wf[:, kc, :], kc == 0, kc == KC - 1,
                       (("hT", b // 4), "wf"), (("ps", 7),))
                tt(fpre[:, b * 8:(b + 1) * 8], ps[7][:, b * 8:(b + 1) * 8], prm_l[:, 34:42], ALU.add,
                   (("ps", 7), "prm"), ("fpre",))
            act(fe, fpre, AF.Exp, ("fpre",), ("fe",), scale=-1.0)
            act(fl, fe, AF.Ln, ("fe",), ("fl",), bias=1.0)
            mm(ps[7][:, 0:NB * 8], trile_f, fl, True, True, ("fl", "c_trile"), (("ps", 7),))
            mm(ps[6][:, 0:NB * 8], ones_f, fl, True, True, ("fl", "c_onesf"), (("ps", 6),))
            cp(tot, ps[6][:, 0:NB * 8], (("ps", 6),), ("tot",))
            add("dve", lambda h_: h_.memset(off[:, 0:8], 0.0), (), ("off",))
            for b in range(NB):
                tt(off[:, (b + 1) * 8:(b + 2) * 8], off[:, b * 8:(b + 1) * 8], tot[:, b * 8:(b + 1) * 8], ALU.add,
                   ("off", "tot"), ("off",))
            tt(G, ps[7][:, 0:NB * 8], off[:, 0:NB * 8], ALU.add, (("ps", 7), "off"), ("G",))
            G3 = G.rearrange("p (b h) -> p b h", h=NH)
            for c in range(NCH):
                nkb = 4 * (c + 1)
                for h in range(NH):
                    col = (4 * c + 2) * 8 + h
                    ts(biasT[c][:, h, 0:nkb], G3[:, 0:nkb, h], off[:, col:col + 1], ALU.subtract, ("G", "off"), ("biasT",))
            load_qk_w(0, 1536, 2048)
            for g in range(4):
                if g + 1 < 4:
                    load_qk_w(g + 1, 1536, 2048)
                proj_qk_fx(g)
                attn_fx_pair(g)
            A.release(MIX_TOP)
            Sx.barrier()

            wgate = A.bf(KC * 2048).rearrange("p (k n) -> p k n", k=KC)
            wosb = A.bf(4 * D).rearrange("p (h n) -> p h n", h=4)
            wofx = A.bf(4 * D).rearrange("p (h n) -> p h n", h=4)
            wout = A.bf(KC * D).rearrange("p (k n) -> p k n", k=KC)
            merged = A.bf(KC * CH).rearrange("p (k n) -> p k n", k=KC)
            xb3 = A.f32(KC * CH).rearrange("p (k n) -> p k n", k=KC)
            ysbc = A.bf(4 * CH).rearrange("p (h n) -> p h n", h=4)
            yfxc = A.bf(4 * CH).rearrange("p (h n) -> p h n", h=4)
            gs = A.f32(CH)
            gf = A.f32(CH)
            m1 = A.f32(CH)
            m2 = A.f32(CH)
            stg_alloc(KC * 128, 3)
            def ld_gate(q):
                wload(wgate[:, :, q * 128:(q + 1) * 128],
                      w_in_l[:, 3080 + q * 128:3080 + (q + 1) * 128].rearrange("(k p) n -> p k n", p=128), ("wgate", q), KC, 128)

            def ld_o(q):
                wload(wosb[:, :, q * 256:(q + 1) * 256],
                      w_o_sb[l][:, q * 256:(q + 1) * 256].rearrange("(h p) n -> p h n", p=128), ("wosb", q), 4, 256)
                wload(wofx[:, :, q * 256:(q + 1) * 256],
                      w_o_fox[l][:, q * 256:(q + 1) * 256].rearrange("(h p) n -> p h n", p=128), ("wofx", q), 4, 256)

            for j_ in range(KC):
                ld_gate(j_)
                ld_gate(8 + j_)
                if j_ % 2 == 0:
                    ld_o(j_ // 2)
            for q in range(8):
                wload(wout[:, :, q * 128:(q + 1) * 128],
                      w_out[l][:, q * 128:(q + 1) * 128].rearrange("(k p) n -> p k n", p=128), ("wout", q), KC, 128)
            for c in range(NCH):
                dma("sp", ysbc, ysb_scr[:, :, c * CH:(c + 1) * CH].rearrange("h d n -> d h n"), (), ("ysbc",))
                dma("sp", yfxc, yfx_scr[:, :, c * CH:(c + 1) * CH].rearrange("h d n -> d h n"), (), ("yfxc",))
                dma("sp", xb3, chunk_src(x_src, c), (), ("xb3",))
                for j in range(KC):
                    for kc in range(KC):
                        mm(ps[0], wgate[:, kc, j * 128:(j + 1) * 128], hT[:, kc, c * CH:(c + 1) * CH], kc == 0, kc == KC - 1,
                           (("wgate", j), ("hT", c)), (("ps", 0),))
                    act(gs, ps[0], AF.Sigmoid, (("ps", 0), "prm"), ("gs",), bias=prm_l[:, 16 + j:17 + j])
                    for kc in range(KC):
                        mm(ps[1], wgate[:, kc, 1024 + j * 128:1024 + (j + 1) * 128], hT[:, kc, c * CH:(c + 1) * CH],
                           kc == 0, kc == KC - 1, (("wgate", 8 + j), ("hT", c)), (("ps", 1),))
                    act(gf, ps[1], AF.Sigmoid, (("ps", 1), "prm"), ("gf",), bias=prm_l[:, 24 + j:25 + j])
                    for h in range(4):
                        mm(ps[2], wosb[:, h, j * 128:(j + 1) * 128], ysbc[:, h, :], h == 0, h == 3,
                           (("wosb", j // 2), "ysbc"), (("ps", 2),))
                    tt(m1, ps[2], gs, ALU.mult, (("ps", 2), "gs"), ("m1",))
                    for h in range(4):
                        mm(ps[3], wofx[:, h, j * 128:(j + 1) * 128], yfxc[:, h, :], h == 0, h == 3,
                           (("wofx", j // 2), "yfxc"), (("ps", 3),))
                    tt(m2, ps[3], gf, ALU.mult, (("ps", 3), "gf"), ("m2",))
                    tt(merged[:, j, :], m1, m2, ALU.add, ("m1", "m2"), (("mg", j),))
                for d in range(KC):
                    pb = 4 + d % 2
                    for j in range(KC):
                        mm(ps[pb], wout[:, j, d * 128:(d + 1) * 128], merged[:, j, :], j == 0, j == KC - 1,
                           (("wout", d), ("mg", j)), (("ps", pb),))
                    tt(xb3[:, d, :], ps[pb], xb3[:, d, :], ALU.add, (("ps", pb), "xb3"), ("xb3",))
                dma("sp", chunk_src(x1T, c), xb3, ("xb3",), ())
            A.release(CONST_TOP)
            Sx.barrier()

            moe = (l % 2 == 1)
            if not moe:
                n_exp = 2

                def gcols(e, fj):
                    return w_gu_dense[0][:, e * FE + fj * 128:e * FE + (fj + 1) * 128]

                def ucols(e, fj):
                    return w_gu_dense[0][:, DFF + e * FE + fj * 128:DFF + e * FE + (fj + 1) * 128]

                def dnrows(e, d):
                    return w_dn_dense[0][e * FE:(e + 1) * FE, d * 128:(d + 1) * 128]
            else:
                n_exp = NE

                def gcols(e, fj):
                    return w_gu_exp[0][e][:, fj * 128:(fj + 1) * 128]

                def ucols(e, fj):
                    return w_gu_exp[0][e][:, FE + fj * 128:FE + (fj + 1) * 128]

                def dnrows(e, d):
                    return w_dn_exp[0][e][:, d * 128:(d + 1) * 128]

            h2T = A.bf(KC * SC).rearrange("p (k n) -> p k n", k=KC)
            acc = A.f32(KC * SC).rearrange("p (k n) -> p k n", k=KC)
            aT_w = A.f32(FJ * SC // 2)
            aT = aT_w.bitcast(BF16).rearrange("p (f n) -> p f n", f=FJ)
            cbc = A.f32(SC)
            sg = [A.f32(CH) for _ in range(2)]
            a32 = [A.f32(CH) for _ in range(2)]
            wg = [A.bf(KC * 128).rearrange("p (k n) -> p k n", k=KC) for _ in range(2)]
            wu = [A.bf(KC * 128).rearrange("p (k n) -> p k n", k=KC) for _ in range(2)]
            wdn = [A.bf(FJ * 128).rearrange("p (f n) -> p f n", f=FJ) for _ in range(2)]
            tagp = {"lnv": A.f32(CH), "rstd": A.f32(CH)}
            assert FJ * SC // 2 >= KC * CH // 2 + KC * CH
            sq = aT_w[:, 0:KC * CH // 2].bitcast(BF16).rearrange("p (k n) -> p k n", k=KC)
            h2f = aT_w[:, KC * CH // 2:KC * CH // 2 + KC * CH].rearrange("p (k n) -> p k n", k=KC)
            wr = A.f32(KC * NE).rearrange("p (k n) -> p k n", k=KC)
            lg = A.f32(BPS * NE)
            comb = A.f32(BPS * NE)
            t1 = A.f32(NE)
            t2 = A.f32(NE)
            mx1 = A.f32(1)
            mx2 = A.f32(1)
            nmx1 = A.f32(1)
            den = A.f32(1)
            rden = A.f32(1)
            diag = [A.f32(128) for _ in range(2)]
            stg_alloc(FJ * 128, 3)
            if moe:
                dma("sp", wr, w_router[0].rearrange("(k p) n -> p k n", p=128), (), ("wr",))
            wcnt = {"gu": 0, "dn": 0, "G": 0, "D": 0, "s": 0, "dg": 0}
            for sc in range(NSC):
                for cc in range(CPS):
                    c = sc * CPS + cc
                    dma("sp", acc[:, :, cc * CH:(cc + 1) * CH], chunk_src(x1T, c), (), (("acc", cc),))
                for cc in range(CPS):
                    rms_chunk(acc[:, :, cc * CH:(cc + 1) * CH], ("acc", cc), sq, 8, prm_l,
                              lambda kc, cc=cc: (h2T[:, kc, cc * CH:(cc + 1) * CH], ("h2T", cc)), tagp,
                              want_f32=(lambda kc: (h2f[:, kc, :], "h2f")) if moe else None)
                    if moe:
                        for bb in range(4):
                            b = cc * 4 + bb
                            for kc in range(KC):
                                mm(ps[6][:, 0:NE], h2f[:, kc, bb * 128:(bb + 1) * 128], wr[:, kc, :], kc == 0, kc == KC - 1,
                                   ("h2f", "wr"), (("ps", 6),))
                            lgb = lg[:, b * NE:(b + 1) * NE]
                            cb = comb[:, b * NE:(b + 1) * NE]
                            cp(lgb, ps[6][:, 0:NE], (("ps", 6),), ("lg",))
                            add("dve", lambda h_, lgb=lgb: h_.tensor_reduce(out=mx1, in_=lgb, axis=AX.X, op=ALU.max), ("lg",), ("mx1",))
                            ts(t1, lgb, mx1, ALU.is_equal, ("lg", "mx1"), ("t1",), s2=-1e30, op1=ALU.mult)
                            tt(t1, t1, lgb, ALU.add, ("t1", "lg"), ("t1",))
                            add("dve", lambda h_: h_.tensor_reduce(out=mx2, in_=t1, axis=AX.X, op=ALU.max), ("t1",), ("mx2",))
                            ts(t2, lgb, mx2, ALU.is_ge, ("lg", "mx2"), ("t2",))
                            ts(nmx1, mx1, -1.0, ALU.mult, ("mx1",), ("nmx1",))
                            act(t1, lgb, AF.Exp, ("lg", "nmx1"), ("t1",), bias=nmx1)
                            tt(t1, t1, t2, ALU.mult, ("t1", "t2"), ("t1",))
                            add("dve", lambda h_: h_.tensor_reduce(out=den, in_=t1, axis=AX.X, op=ALU.add), ("t1",), ("den",))
                            add("dve", lambda h_: h_.reciprocal(out=rden, in_=den), ("den",), ("rden",))
                            ts(cb, t1, rden, ALU.mult, ("t1", "rden"), ("comb",))
                Sx.barrier()
                for e in range(n_exp):
                    if moe:
                        for cc in range(CPS):
                            for bb in range(4):
                                b = cc * 4 + bb
                                dg = diag[wcnt["dg"] % 2]
                                dk = ("diag", wcnt["dg"] % 2)
                                wcnt["dg"] += 1
                                ts(dg, ident_f, comb[:, b * NE + e:b * NE + e + 1], ALU.mult, ("comb", "c_ident"), (dk,))
                                mm(ps[7][:, bb * 128:(bb + 1) * 128], ones_f, dg, True, True, (dk, "c_onesf"), (("ps", 7),))
                            cp(cbc[:, cc * CH:(cc + 1) * CH], ps[7], (("ps", 7),), (("cbc", cc),))
                    for fj in range(FJ):
                        wp = wcnt["gu"] % 2
                        wcnt["gu"] += 1
                        wload(wg[wp], gcols(e, fj).rearrange("(k p) n -> p k n", p=128), ("wg", wp), KC, 128)
                        wload(wu[wp], ucols(e, fj).rearrange("(k p) n -> p k n", p=128), ("wu", wp), KC, 128)
                        for cc in range(CPS):
                            gb = wcnt["G"] % 2
                            wcnt["G"] += 1
                            sp_ = wcnt["s"] % 2
                            wcnt["s"] += 1
                            for kc in range(KC):
                                mm(ps[gb], wg[wp][:, kc, :], h2T[:, kc, cc * CH:(cc + 1) * CH], kc == 0, kc == KC - 1,
                                   (("wg", wp), ("h2T", cc)), (("ps", gb),))
                            for kc in range(KC):
                                mm(ps[2 + gb], wu[wp][:, kc, :], h2T[:, kc, cc * CH:(cc + 1) * CH], kc == 0, kc == KC - 1,
                                   (("wu", wp), ("h2T", cc)), (("ps", 2 + gb),))
                            act(sg[sp_], ps[gb], AF.Silu, (("ps", gb),), (("sg", sp_),))
                            if moe:
                                tt(a32[sp_], sg[sp_], ps[2 + gb], ALU.mult, (("sg", sp_), ("ps", 2 + gb)), (("a32", sp_),))
                                tt(aT[:, fj, cc * CH:(cc + 1) * CH], a32[sp_], cbc[:, cc * CH:(cc + 1) * CH], ALU.mult,
                                   (("a32", sp_), ("cbc", cc)), (("aT", fj, cc),))
                            else:
                                tt(aT[:, fj, cc * CH:(cc + 1) * CH], sg[sp_], ps[2 + gb], ALU.mult,
                                   (("sg", sp_), ("ps", 2 + gb)), (("aT", fj, cc),))
                    for d in range(KC):
                        wp = wcnt["dn"] % 2
                        wcnt["dn"] += 1
                        wload(wdn[wp], dnrows(e, d).rearrange("(f p) n -> p f n", p=128), ("wdn", wp), FJ, 128)
                        for cc in range(CPS):
                            db = 4 + wcnt["D"] % 2
                            wcnt["D"] += 1
                            for fj in range(FJ):
                                mm(ps[db], wdn[wp][:, fj, :], aT[:, fj, cc * CH:(cc + 1) * CH], fj == 0, fj == FJ - 1,
                                   (("wdn", wp), ("aT", fj, cc)), (("ps", db),))
                            tt(acc[:, d, cc * CH:(cc + 1) * CH], ps[db], acc[:, d, cc * CH:(cc + 1) * CH], ALU.add,
                               (("ps", db), ("acc", cc)), (("acc", cc),))
                for cc in range(CPS):
                    c = sc * CPS + cc
                    dma("sp", chunk_src(x_dst, c), acc[:, :, cc * CH:(cc + 1) * CH], (("acc", cc),), ())
                Sx.barrier()
            A.release(CONST_TOP)
        Sx.finish()
        Sx.emit()
    return nc


_NC_CACHE = {}


def _prep_inputs(inputs, S):
    x = np.asarray(inputs["x"], dtype=np.float32)
    B = x.shape[0]
    g_mix = np.asarray(inputs["g_mix"], np.float32)
    g_ffn = np.asarray(inputs["g_ffn"], np.float32)
    b_gate = np.asarray(inputs["b_gate"], np.float32)
    g_q = np.asarray(inputs["g_q"], np.float32)
    g_k = np.asarray(inputs["g_k"], np.float32)
    b_f = np.asarray(inputs["b_f"], np.float32)
    prm = np.zeros((2, 128, 64), np.float32)
    for l in range(2):
        prm[l, :, 0:8] = g_mix[l].reshape(8, 128).T
        prm[l, :, 8:16] = g_ffn[l].reshape(8, 128).T
        prm[l, :, 16:32] = b_gate[l].reshape(16, 128).T
        prm[l, :, 32] = np.tile(g_q[l], 2)
        prm[l, :, 33] = np.tile(g_k[l], 2)
        prm[l, :, 34:42] = np.broadcast_to(b_f[l][None, :], (128, 8))
    shared = {"prm": prm}
    for k in ("w_in", "w_o_sb", "w_o_fox", "w_out", "w_gu_dense", "w_dn_dense", "w_router", "w_gu_exp", "w_dn_exp"):
        shared[k] = np.ascontiguousarray(np.asarray(inputs[k], np.float32))
    in_maps = []
    for b in range(B):
        m = dict(shared)
        m["xT"] = np.ascontiguousarray(x[b].T)
        in_maps.append(m)
    return in_maps


def kernel(**inputs):
    x = inputs["x"]
    B, S, _ = x.shape
    key = (S,)
    if key not in _NC_CACHE:
        _NC_CACHE[key] = build_nc(S)
    nc = _NC_CACHE[key]
    in_maps = _prep_inputs(inputs, S)
    res = run_bass_kernel_spmd(nc, in_maps, core_ids=list(range(B)))
    out = np.stack([np.ascontiguousarray(res.results[b]["outT"].T) for b in range(B)], axis=0)
    return out.astype(np.float32)
```

```python
from contextlib import ExitStack
import numpy as np
import concourse.bass as bass
import concourse.mybir as mybir
from concourse.bass_utils import run_bass_kernel_spmd

F32 = mybir.dt.float32
BF16 = mybir.dt.bfloat16
I32 = mybir.dt.int32
AF = mybir.ActivationFunctionType
ALU = mybir.AluOpType
AX = mybir.AxisListType

D = 1024
KC = 8
HD = 64
NH = 8
PROJ_IN = 5128
DFF = 2816
NE = 8
FE = 1408
FJ = 11
EPS = 1e-6
CH = 512

SEM_LIMIT = 20000
N_DMA_SEMS = 24


class Op:
    __slots__ = ("eng", "fn", "deps", "dma", "milestone", "sem", "count", "dsem", "dcount", "dprev")

    def __init__(self, eng, fn, deps, dma):
        self.eng = eng
        self.fn = fn
        self.deps = deps
        self.dma = dma
        self.milestone = False
        self.sem = None
        self.count = 0
        self.dsem = None
        self.dcount = 0
        self.dprev = 0


class Sched:
    ENGS = ("pe", "act", "dve", "pool", "sp")

    def __init__(self, nc, same_engine_sync=True):
        self.nc = nc
        self.ops = []
        self.lastw = {}
        self.rd = {}
        self.same = same_engine_sync
        self.last_on = {e: None for e in self.ENGS}
        self.pending_barrier = {e: None for e in self.ENGS}
        self.dma_since = []

    def add(self, eng, fn, reads=(), writes=(), dma=False):
        idx = len(self.ops)
        deps = set()
        for k in reads:
            w = self.lastw.get(k)
            if w is not None:
                deps.add(w)
        for k in writes:
            w = self.lastw.get(k)
            if w is not None:
                deps.add(w)
            for r in self.rd.get(k, ()):
                deps.add(r)
        for k in reads:
            lst = self.rd.setdefault(k, [])
            if not dma:
                lst[:] = [r for r in lst if self.ops[r].dma or self.ops[r].eng != eng]
            lst.append(idx)
        for k in writes:
            self.lastw[k] = idx
            self.rd[k] = []
        pb = self.pending_barrier[eng]
        if pb is not None:
            deps |= pb
            self.pending_barrier[eng] = None
        deps.discard(idx)
        self.ops.append(Op(eng, fn, deps, dma))
        self.last_on[eng] = idx
        if dma:
            self.dma_since.append(idx)
        return idx

    def barrier(self):
        deps = set(i for i in self.last_on.values() if i is not None)
        deps |= set(self.dma_since)
        self.dma_since = []
        for e in self.ENGS:
            cur = self.pending_barrier[e]
            self.pending_barrier[e] = (cur | deps) if cur is not None else set(deps)

    def finish(self):
        self.barrier()
        self.add("sp", None)

    def emit(self):
        nc = self.nc
        ops = self.ops
        for op in ops:
            for d in op.deps:
                p = ops[d]
                if p.dma:
                    continue
                if p.eng != op.eng:
                    p.milestone = True
                elif self.same and op.eng in ("act", "dve", "pool"):
                    p.milestone = True
        n_epochs = {e: 1 for e in self.ENGS}
        cnt = {e: 0 for e in self.ENGS}
        ep = {e: 0 for e in self.ENGS}
        dma_uses = [0] * N_DMA_SEMS
        dma_rr = 0
        for op in ops:
            if op.dma:
                k = dma_rr % N_DMA_SEMS
                dma_rr += 1
                op.dsem = k
                op.dprev = 16 * dma_uses[k]
                dma_uses[k] += 1
                op.dcount = 16 * dma_uses[k]
            elif op.milestone:
                e = op.eng
                if cnt[e] >= SEM_LIMIT:
                    ep[e] += 1
                    cnt[e] = 0
                    n_epochs[e] = ep[e] + 1
                cnt[e] += 1
                op.sem = (e, ep[e])
                op.count = cnt[e]
        with ExitStack() as st:
            sems = {}
            for e in self.ENGS:
                for k in range(n_epochs[e]):
                    sems[(e, k)] = st.enter_context(nc.semaphore(f"s_{e}_{k}"))
            dsems = [st.enter_context(nc.semaphore(f"s_dma_{k}")) for k in range(N_DMA_SEMS)]
            block = st.enter_context(nc.Block())

            def run_engine(eng_name, handle):
                waited = {}
                for op in ops:
                    if op.eng != eng_name:
                        continue
                    need = {}
                    for d in op.deps:
                        p = ops[d]
                        if p.dma:
                            key = ("d", p.dsem)
                            val = p.dcount
                        else:
                            if not p.milestone:
                                continue
                            if p.eng == eng_name and not (self.same and eng_name in ("act", "dve", "pool")):
                                continue
                            key = p.sem
                            val = p.count
                        if need.get(key, 0) < val:
                            need[key] = val
                    if op.dma and op.dprev > 0:
                        key = ("d", op.dsem)
                        if need.get(key, 0) < op.dprev:
                            need[key] = op.dprev
                    for key, val in need.items():
                        if key[0] == "d":
                            if waited.get(key, 0) >= val:
                                continue
                            handle.wait_ge(dsems[key[1]], val)
                            waited[key] = val
                        else:
                            e, k = key
                            w = waited.get(e, (-1, 0))
                            if w[0] > k or (w[0] == k and w[1] >= val):
                                continue
                            handle.wait_ge(sems[key], val)
                            waited[e] = (k, val)
                    if op.fn is None:
                        continue
                    ins = op.fn(handle)
                    if op.dma:
                        ins.then_inc(dsems[op.dsem], 16)
                    elif op.milestone:
                        ins.then_inc(sems[op.sem], 1)

            @block.tensor
            def _(h):
                run_engine("pe", h)

            @block.scalar
            def _(h):
                run_engine("act", h)

            @block.vector
            def _(h):
                run_engine("dve", h)

            @block.gpsimd
            def _(h):
                run_engine("pool", h)

            @block.sync
            def _(h):
                run_engine("sp", h)


class Arena:
    def __init__(self, ar, words):
        self.ar = ar
        self.words = words
        self.top = 0

    def mark(self):
        return self.top

    def release(self, m):
        self.top = m

    def f32(self, n, parts=128):
        o = self.top
        self.top += n
        assert self.top <= self.words, f"arena overflow {self.top}"
        return self.ar[0:parts, o:o + n]

    def bf(self, n, parts=128):
        w = (n + 1) // 2
        o = self.top
        self.top += w
        assert self.top <= self.words, f"arena overflow {self.top}"
        return self.ar[0:parts, o:o + w].bitcast(BF16)[:, 0:n]

    def i32(self, n):
        o = self.top
        self.top += n
        assert self.top <= self.words
        return self.ar[:, o:o + n].bitcast(I32)


def build_nc(S, n_layers=2, same_engine_sync=True):
    NCH = S // CH
    NB = S // 128
    SC = min(2048, S)
    NSC = S // SC
    CPS = SC // CH
    BPS = SC // 128

    nc = bass.Bass("TRN2", target_bir_lowering=False)
    dr = {}

    def din(name, shape, dt=F32):
        dr[name] = nc.dram_tensor(name, list(shape), dt, kind="ExternalInput").ap()
        return dr[name]

    xT_in = din("xT", [D, S])
    prm_in = din("prm", [2, 128, 64])
    w_in = din("w_in", [2, D, PROJ_IN])
    w_o_sb = din("w_o_sb", [2, 512, D])
    w_o_fox = din("w_o_fox", [2, 512, D])
    w_out = din("w_out", [2, D, D])
    w_gu_dense = din("w_gu_dense", [1, D, 2 * DFF])
    w_dn_dense = din("w_dn_dense", [1, DFF, D])
    w_router = din("w_router", [1, D, NE])
    w_gu_exp = din("w_gu_exp", [1, NE, D, 2 * FE])
    w_dn_exp = din("w_dn_exp", [1, NE, FE, D])
    outT = nc.dram_tensor("outT", [D, S], F32, kind="ExternalOutput").ap()
    x1T = nc.dram_tensor("x1T_scr", [D, S], F32, kind="Internal").ap()
    xmidT = nc.dram_tensor("xmidT_scr", [D, S], F32, kind="Internal").ap()
    ysb_scr = nc.dram_tensor("ysb_scr", [4, 128, S], BF16, kind="Internal").ap()
    yfx_scr = nc.dram_tensor("yfx_scr", [4, 128, S], BF16, kind="Internal").ap()

    WORDS = 53000
    with ExitStack() as st:
        ar_t = st.enter_context(nc.sbuf_tensor("arena", [128, WORDS], F32))
        psall = st.enter_context(nc.psum_tensor("psall", [128, 8 * 512], F32))[:, :]
        ps = [psall[:, b * 512:(b + 1) * 512] for b in range(8)]

        def ps2(b):
            return psall[:, b * 512:(b + 2) * 512].rearrange("p (i n) -> p i n", i=2)
        A = Arena(ar_t, WORDS)
        Sx = Sched(nc, same_engine_sync=same_engine_sync)
        add = Sx.add

        def mm(out, lhsT, rhs, start, stop, reads, writes, sgc=False):
            if sgc:
                add("pe", lambda h: h.matmul(out, lhsT, rhs, start=start, stop=stop, skip_group_check=True), reads, writes)
            else:
                add("pe", lambda h: h.matmul(out, lhsT, rhs, start=start, stop=stop), reads, writes)

        def act(out, in_, func, reads, writes, scale=1.0, bias=None):
            if bias is None:
                add("act", lambda h: h.activation(out=out, in_=in_, func=func, scale=scale), reads, writes)
            else:
                add("act", lambda h: h.activation(out=out, in_=in_, func=func, scale=scale, bias=bias), reads, writes)

        def tt(out, in0, in1, op, reads, writes, eng="dve"):
            add(eng, lambda h: h.tensor_tensor(out=out, in0=in0, in1=in1, op=op), reads, writes)

        def ts(out, in0, s1, op0, reads, writes, s2=None, op1=None, eng="dve"):
            if op1 is None:
                add(eng, lambda h: h.tensor_scalar(out=out, in0=in0, scalar1=s1, scalar2=None, op0=op0), reads, writes)
            else:
                add(eng, lambda h: h.tensor_scalar(out=out, in0=in0, scalar1=s1, scalar2=s2, op0=op0, op1=op1), reads, writes)

        def stt(out, in0, scalar, in1, op0, op1, reads, writes):
            add("dve", lambda h: h.scalar_tensor_tensor(out=out, in0=in0, scalar=scalar, in1=in1, op0=op0, op1=op1), reads, writes)

        def cp(out, in_, reads, writes, eng="dve"):
            add(eng, lambda h: h.tensor_copy(out=out, in_=in_), reads, writes)

        def dma(q, out, in_, reads, writes):
            add(q, lambda h: h.dma_start(out=out, in_=in_), reads, writes, dma=True)

        stg = {"bufs": [], "n": 0}

        def stg_alloc(words, n):
            stg["bufs"] = [A.f32(words) for _ in range(n)]

        def wload(dst, src, wkey, a, b, parts=128):
            k = stg["n"] % len(stg["bufs"])
            stg["n"] += 1
            sv = stg["bufs"][k][0:parts, 0:a * b].rearrange("p (a b) -> p a b", a=a)
            dma("sp", sv, src, (), (("stg", k),))
            cp(dst, sv, (("stg", k),), (wkey,), eng="pool")

        ones_bf = A.bf(128)
        negtri_bf = A.bf(128)
        bdiag_bf = A.bf(128)
        ident_f = A.f32(128)
        ones_f = A.f32(128)
        trile_f = A.f32(128)
        maskSB = [A.bf(512) for _ in range(4)]
        maskFX = [A.bf(512) for _ in range(4)]
        prm = [A.f32(64) for _ in range(2)]
        io_t = A.i32(512)

        add("dve", lambda h: h.memset(ones_bf, 1.0), (), ("c_ones",))
        add("dve", lambda h: h.memset(ones_f, 1.0), (), ("c_onesf",))
        add("dve", lambda h: h.memset(bdiag_bf, 0.0), (), ("c_bdiag",))
        add("dve", lambda h: h.memset(bdiag_bf[0:64, 0:64], 1.0), (), ("c_bdiag",))
        add("dve", lambda h: h.memset(bdiag_bf[64:128, 64:128], 1.0), (), ("c_bdiag",))
        add("pool", lambda h: h.iota(io_t[:, 0:128], pattern=[[-1, 128]], base=0, channel_multiplier=1), (), ("iota",))
        ts(negtri_bf, io_t[:, 0:128], 0.0, ALU.is_ge, ("iota",), ("c_negtri",), s2=-1.0, op1=ALU.mult)
        ts(trile_f, io_t[:, 0:128], 0.0, ALU.is_le, ("iota",), ("c_trile",))
        ts(ident_f, io_t[:, 0:128], 0.0, ALU.is_equal, ("iota",), ("c_ident",))
        for j in range(4):
            add("pool", lambda h, j=j: h.iota(io_t[:, 0:512], pattern=[[1, 512]], base=-128 * j, channel_multiplier=-1),
                ("iota",), ("iota",))
            ts(maskSB[j], io_t[:, 0:512], 0.0, ALU.is_gt, ("iota",), ("c_mask",))
            ts(maskFX[j], io_t[:, 0:512], 0.0, ALU.is_ge, ("iota",), ("c_mask",))
        for l in range(2):
            dma("sp", prm[l], prm_in[l], (), ("prm", ))
        CONST_TOP = A.mark()

        def chunk_src(src, c):
            return src[:, c * CH:(c + 1) * CH].rearrange("(k p) n -> p k n", p=128)

        def rms_chunk(xb, xkey, sq, gcol0, prm_l, out_fn, tagp, want_f32=None):
            lnv = tagp["lnv"]
            rstd = tagp["rstd"]
            act(sq, xb, AF.Square, (xkey,), ("sq",))
            for kc in range(KC):
                mm(ps[7], ones_bf, sq[:, kc, :], kc == 0, kc == KC - 1, ("sq", "c_ones"), (("ps", 7),))
            act(lnv, ps[7], AF.Ln, (("ps", 7),), ("lnv",), scale=1.0 / D, bias=EPS)
            act(rstd, lnv, AF.Exp, ("lnv",), ("rstd",), scale=-0.5)
            for kc in range(KC):
                o, okey = out_fn(kc)
                stt(o, xb[:, kc, :], prm_l[:, gcol0 + kc:gcol0 + kc + 1], rstd, ALU.mult, ALU.mult,
                    (xkey, "rstd", "prm"), (okey,))
                if want_f32 is not None:
                    o2, o2key = want_f32(kc)
                    stt(o2, xb[:, kc, :], prm_l[:, gcol0 + kc:gcol0 + kc + 1], rstd, ALU.mult, ALU.mult,
                        (xkey, "rstd", "prm"), (o2key,))

        for l in range(n_layers):
            prm_l = prm[l]
            w_in_l = w_in[l]
            x_src = xT_in if l == 0 else xmidT
            x_dst = outT if l == n_layers - 1 else xmidT
            A.release(CONST_TOP)
            Sx.barrier()
            hT = A.bf(KC * S).rearrange("p (k n) -> p k n", k=KC)
            MIX_TOP = A.mark()
            xb = [A.f32(KC * CH).rearrange("p (k n) -> p k n", k=KC) for _ in range(2)]
            sq = A.bf(KC * CH).rearrange("p (k n) -> p k n", k=KC)
            tagp = {"lnv": A.f32(CH), "rstd": A.f32(CH)}
            dma("sp", xb[0], chunk_src(x_src, 0), (), (("xb", 0),))
            for c in range(NCH):
                if c + 1 < NCH:
                    dma("sp", xb[(c + 1) % 2], chunk_src(x_src, c + 1), (), (("xb", (c + 1) % 2),))
                rms_chunk(xb[c % 2], ("xb", c % 2), sq, 0, prm_l,
                          lambda kc, c=c: (hT[:, kc, c * CH:(c + 1) * CH], ("hT", c)), tagp)
            A.release(MIX_TOP)
            Sx.barrier()

            V = A.bf(NB * 512).rearrange("p (b n) -> p b n", b=NB)
            regA = A.f32(2 * 2 * CH)
            regB = A.f32(2 * 2 * CH)
            wv = regA.bitcast(BF16).rearrange("p (k n) -> p k n", k=KC)
            qTh = [A.bf(S), A.bf(S)]
            kT = A.bf(S)
            wq = [A.bf(KC * 128).rearrange("p (k n) -> p k n", k=KC) for _ in range(2)]
            wk = [A.bf(KC * 128).rearrange("p (k n) -> p k n", k=KC) for _ in range(2)]
            e2 = [regA[:, k_ * 2 * CH:(k_ + 1) * 2 * CH].rearrange("p (i n) -> p i n", i=2) for k_ in range(2)]
            L2 = [A.bf(2 * CH).rearrange("p (i n) -> p i n", i=2) for _ in range(3)]
            w2 = [A.bf(2 * CH).rearrange("p (i n) -> p i n", i=2) for _ in range(3)]
            tmp2 = [regB[:, k_ * 2 * CH:(k_ + 1) * 2 * CH].rearrange("p (i n) -> p i n", i=2) for k_ in range(2)]
            R2 = A.f32(2 * CH).rearrange("p (i n) -> p i n", i=2)
            w_sb = [A.bf(CH) for _ in range(4)]
            yo = [A.bf(CH) for _ in range(2)]
            rec = [A.f32(CH) for _ in range(2)]
            qraw = A.f32(CH)
            sq2 = A.bf(CH)
            lnv2 = A.f32(CH)
            r2 = A.f32(CH)
            wf = A.bf(KC * 8).rearrange("p (k n) -> p k n", k=KC)
            fpre = A.f32(NB * 8)
            fe = A.f32(NB * 8)
            fl = A.f32(NB * 8)
            tot = A.f32(NB * 8)
            G = A.f32(NB * 8)
            off = A.f32((NB + 1) * 8)
            assert NCH * NH * NB <= 2 * 2 * CH
            biasT = [regB[:, c_ * NH * NB:(c_ + 1) * NH * NB].rearrange("p (h b) -> p h b", h=NH) for c_ in range(NCH)]
            cnt = {"P": 0}
            stg_alloc(KC * 128, 2)

            add("dve", lambda h_: h_.memset(qTh[0][64:128, :], 0.0), (), ("qz0",))
            add("dve", lambda h_: h_.memset(qTh[1][0:64, :], 0.0), (), ("qz1",))

            def proj_V(col0):
                for q4 in range(4):
                    wload(wv[:, :, q4 * 128:(q4 + 1) * 128],
                          w_in_l[:, col0 + q4 * 128:col0 + (q4 + 1) * 128].rearrange("(k p) n -> p k n", p=128), "wv", KC, 128)
                for b in range(NB):
                    pbi = b % 4
                    pb = ps[pbi]
                    for kc in range(KC):
                        mm(pb, hT[:, kc, b * 128:(b + 1) * 128], wv[:, kc, :], kc == 0, kc == KC - 1,
                           (("hT", b // 4), "wv"), (("ps", pbi),))
                    if b % 2 == 0:
                        act(V[:, b, :], pb, AF.Copy, (("ps", pbi),), (("V", b),))
                    else:
                        cp(V[:, b, :], pb, (("ps", pbi),), (("V", b),))

            def load_qk_w(g, qcol0, kcol0):
                par = g % 2
                wload(wq[par], w_in_l[:, qcol0 + g * 128:qcol0 + (g + 1) * 128].rearrange("(k p) n -> p k n", p=128),
                      ("wq", par), KC, 128)
                wload(wk[par], w_in_l[:, kcol0 + g * 128:kcol0 + (g + 1) * 128].rearrange("(k p) n -> p k n", p=128),
                      ("wk", par), KC, 128)

            def proj_qk_sb(g):
                par = g % 2
                for c in range(NCH):
                    cs = slice(c * CH, (c + 1) * CH)
                    pb = cnt["P"] % 4
                    cnt["P"] += 1
                    for kc in range(KC):
                        mm(ps[pb], wq[par][:, kc, :], hT[:, kc, cs], kc == 0, kc == KC - 1, (("hT", c), ("wq", par)), (("ps", pb),))
                    ts(qTh[0][0:64, cs], ps[pb][0:64, :], 0.125, ALU.mult, (("ps", pb), "qz0"), (("qT", 0, c),))
                    ts(qTh[1][64:128, cs], ps[pb][64:128, :], 0.125, ALU.mult, (("ps", pb), "qz1"), (("qT", 1, c),))
                    pb = cnt["P"] % 4
                    cnt["P"] += 1
                    for kc in range(KC):
                        mm(ps[pb], wk[par][:, kc, :], hT[:, kc, cs], kc == 0, kc == KC - 1, (("hT", c), ("wk", par)), (("ps", pb),))
                    cp(kT[:, cs], ps[pb], (("ps", pb),), (("kT", c),))

            def proj_qk_fx(g):
                par = g % 2
                for c in range(NCH):
                    cs = slice(c * CH, (c + 1) * CH)
                    for isq in (True, False):
                        wt, wkey = (wq[par], ("wq", par)) if isq else (wk[par], ("wk", par))
                        pb = cnt["P"] % 4
                        cnt["P"] += 1
                        for kc in range(KC):
                            mm(ps[pb], wt[:, kc, :], hT[:, kc, cs], kc == 0, kc == KC - 1, (("hT", c), wkey), (("ps", pb),))
                        cp(qraw, ps[pb], (("ps", pb),), ("qraw",))
                        act(sq2, qraw, AF.Square, ("qraw",), ("sq2",))
                        mm(ps[6], bdiag_bf, sq2, True, True, ("sq2", "c_bdiag"), (("ps", 6),))
                        act(lnv2, ps[6], AF.Ln, (("ps", 6),), ("lnv2",), scale=1.0 / HD, bias=EPS)
                        if isq:
                            act(r2, lnv2, AF.Exp, ("lnv2",), ("r2",), scale=-0.5, bias=-2.0794415416798357)
                            stt(qTh[0][0:64, cs], qraw[0:64, :], prm_l[0:64, 32:33], r2[0:64, :], ALU.mult, ALU.mult,
                                ("qraw", "r2", "prm", "qz0"), (("qT", 0, c),))
                            stt(qTh[1][64:128, cs], qraw[64:128, :], prm_l[64:128, 32:33], r2[64:128, :], ALU.mult, ALU.mult,
                                ("qraw", "r2", "prm", "qz1"), (("qT", 1, c),))
                        else:
                            act(r2, lnv2, AF.Exp, ("lnv2",), ("r2",), scale=-0.5)
                            stt(kT[:, cs], qraw, prm_l[:, 33:34], r2, ALU.mult, ALU.mult, ("qraw", "r2", "prm"), (("kT", c),))

            def tasks_for(desc):
                tl = []
                for c in range(NCH):
                    nkb = 4 * (c + 1)
                    kbs = list(range(nkb))
                    if desc:
                        kbs = kbs[::-1]
                    for n_, kb in enumerate(kbs):
                        for i in (0, 1):
                            tl.append((c, kb, i, n_ == 0, n_ == nkb - 1))
                return tl

            def attn_sb_pair(g):
                tl = []
                for c in range(NCH):
                    nkb = 4 * (c + 1)
                    for n_, kb in enumerate(reversed(range(nkb))):
                        tl.append((c, kb, n_ == 0, n_ == nkb - 1))
                N = len(tl)
                vs = slice(g * 128, (g + 1) * 128)

                def info(n):
                    c, kb, first, last = tl[n]
                    j = kb - 4 * c
                    c0 = 128 * j if j > 0 else 0
                    return c, kb, first, last, j, c0

                def s1(n):
                    c, kb, first, last, j, c0 = info(n)
                    sl = n % 2
                    Aks = (("ps", 2 * sl), ("ps", 2 * sl + 1))
                    for i in (0, 1):
                        mm(ps[2 * sl + i][:, c0:], kT[:, kb * 128:(kb + 1) * 128], qTh[i][:, c * CH + c0:(c + 1) * CH], True, True,
                           (("kT", kb // 4), ("qT", i, c)), (Aks[i],))
                    act(e2[sl][:, :, c0:], ps2(2 * sl)[:, :, c0:], AF.Exp, Aks, (("e2", sl),))
                    act(L2[n % 3][:, :, c0:], e2[sl][:, :, c0:], AF.Ln, (("e2", sl),), (("L2", n % 3),), bias=1.0)
                    if j >= 0:
                        for i in (0, 1):
                            tt(L2[n % 3][:, i, c0:], L2[n % 3][:, i, c0:], maskSB[j][:, c0:], ALU.mult,
                               (("L2", n % 3), "c_mask"), (("L2", n % 3),), eng="pool")

                def s2(n):
                    c, kb, first, last, j, c0 = info(n)
                    sl = n % 2
                    Aks = (("ps", 2 * sl), ("ps", 2 * sl + 1))
                    Lk = ("L2", n % 3)
                    for i in (0, 1):
                        mm(ps[2 * sl + i][:, c0:], negtri_bf, L2[n % 3][:, i, c0:], False, True, (Lk, "c_negtri"), (Aks[i],), sgc=True)
                    if not last:
                        for i in (0, 1):
                            mm(ps[6 + i][:, c0:], ones_bf, L2[n % 3][:, i, c0:], True, True, (Lk, "c_ones"), (("ps", 6 + i),))
                    wk_ = ("w2", n % 3)
                    if first:
                        act(w2[n % 3][:, :, c0:], ps2(2 * sl)[:, :, c0:], AF.Exp, Aks, (wk_,))
                    else:
                        tt(tmp2[sl][:, :, c0:], ps2(2 * sl)[:, :, c0:], R2[:, :, c0:], ALU.subtract, Aks + ("R2",), (("tmp2", sl),))
                        act(w2[n % 3][:, :, c0:], tmp2[sl][:, :, c0:], AF.Exp, (("tmp2", sl),), (wk_,))
                    if j >= 0:
                        for i in (0, 1):
                            tt(w2[n % 3][:, i, c0:], w2[n % 3][:, i, c0:], maskSB[j][:, c0:], ALU.mult, (wk_, "c_mask"), (wk_,), eng="pool")
                    if not last:
                        Bks = (("ps", 6), ("ps", 7))
                        if first:
                            if c0 > 0:
                                add("dve", lambda h_: h_.memset(R2[:, :, 0:c0], 0.0), (), ("R2",))
                            cp(R2[:, :, c0:], ps2(6)[:, :, c0:], Bks, ("R2",))
                        else:
                            tt(R2[:, :, c0:], ps2(6)[:, :, c0:], R2[:, :, c0:], ALU.add, Bks + ("R2",), ("R2",))

                def s3(n):
                    c, kb, first, last, j, c0 = info(n)
                    for i in (0, 1):
                        ob = 4 + i
                        mm(ps[ob][:, c0:], V[:, kb, vs], w2[n % 3][:, i, c0:], first, last, (("w2", n % 3), ("V", kb)), (("ps", ob),))
                    if last:
                        yb = c % 2
                        cp(yo[yb][0:64, :], ps[4][0:64, :], (("ps", 4),), (("yo", yb, 0),))
                        cp(yo[yb][64:128, :], ps[5][64:128, :], (("ps", 5),), (("yo", yb, 1),))
                        dma("sp", ysb_scr[g, :, c * CH:(c + 1) * CH], yo[yb], (("yo", yb, 0), ("yo", yb, 1)), ())

                for it in range(N + 2):
                    if it < N:
                        s1(it)
                    if 0 <= it - 1 < N:
                        s2(it - 1)
                    if 0 <= it - 2 < N:
                        s3(it - 2)

            def attn_fx_pair(g):
                tl = tasks_for(False)
                N = len(tl)
                vs = slice(g * 128, (g + 1) * 128)

                def s1(n):
                    c, kb, i, first, last = tl[n]
                    ab = n % 4
                    Ak = ("ps", ab)
                    j = kb - 4 * c
                    h = 2 * g + i
                    c0 = 128 * j if j > 0 else 0
                    mm(ps[ab][:, c0:], kT[:, kb * 128:(kb + 1) * 128], qTh[i][:, c * CH + c0:(c + 1) * CH], True, True,
                       (("kT", kb // 4), ("qT", i, c)), (Ak,))
                    act(w_sb[ab][:, c0:], ps[ab][:, c0:], AF.Exp, (Ak, "biasT"), (("w", ab),), bias=biasT[c][:, h, kb:kb + 1])
                    if j >= 0:
                        tt(w_sb[ab][:, c0:], w_sb[ab][:, c0:], maskFX[j][:, c0:], ALU.mult, (("w", ab), "c_mask"), (("w", ab),), eng="pool")

                def s2(n):
                    c, kb, i, first, last = tl[n]
                    ab = n % 4
                    ob = 4 + i
                    db = 6 + i
                    j = kb - 4 * c
                    c0 = 128 * j if j > 0 else 0
                    mm(ps[ob][:, c0:], V[:, kb, vs], w_sb[ab][:, c0:], first, last, (("w", ab), ("V", kb)), (("ps", ob),))
                    mm(ps[db][:, c0:], ones_bf, w_sb[ab][:, c0:], first, last, (("w", ab), "c_ones"), (("ps", db),))
                    if last:
                        yb = c % 2
                        r = slice(64 * i, 64 * i + 64)
                        recf = rec[0]
                        act(yo[yb][r, :], ps[ob][r, :], AF.Copy, (("ps", ob),), (("yo", yb, i),))
                        cp(recf[r, :], ps[db][r, :], (("ps", db),), (("rec", i),))
                        if i == 1:
                            rk = (("rec", 0), ("rec", 1))
                            yk = (("yo", yb, 0), ("yo", yb, 1))
                            add("dve", lambda h_: h_.reciprocal(out=recf, in_=recf), rk, rk)
                            tt(yo[yb], yo[yb], recf, ALU.mult, yk + rk, yk)
                            dma("sp", yfx_scr[g, :, c * CH:(c + 1) * CH], yo[yb], yk, ())

                for it in range(N + 3):
                    if it < N:
                        s1(it)
                    if 0 <= it - 3 < N:
                        s2(it - 3)

            proj_V(1024)
            Sx.barrier()
            load_qk_w(0, 0, 512)
            for g in range(4):
                if g + 1 < 4:
                    load_qk_w(g + 1, 0, 512)
                proj_qk_sb(g)
                attn_sb_pair(g)
            Sx.barrier()
            proj_V(2560)
            Sx.barrier()
            wload(wf, w_in_l[:, 3072:3080].rearrange("(k p) n -> p k n", p=128), "wf", KC, 8)
            for b in range(NB):
                for kc in range(KC):
                    mm(ps[7][:, b * 8:(b + 1) * 8], hT[:, kc, b * 128:(b + 1) * 128], wf[:, kc, :], kc == 0, kc == KC - 1,
                       (("hT", b // 4), "wf"), (("ps", 7),))
                tt(fpre[:, b * 8:(b + 1) * 8], ps[7][:, b * 8:(b + 1) * 8], prm_l[:, 34:42], ALU.add,
                   (("ps", 7), "prm"), ("fpre",))
            act(fe, fpre, AF.Exp, ("fpre",), ("fe",), scale=-1.0)
            act(fl, fe, AF.Ln, ("fe",), ("fl",), bias=1.0)
            mm(ps[7][:, 0:NB * 8], trile_f, fl, True, True, ("fl", "c_trile"), (("ps", 7),))
            mm(ps[6][:, 0:NB * 8], ones_f, fl, True, True, ("fl", "c_onesf"), (("ps", 6),))
            cp(tot, ps[6][:, 0:NB * 8], (("ps", 6),), ("tot",))
            add("dve", lambda h_: h_.memset(off[:, 0:8], 0.0), (), ("off",))
            for b in range(NB):
                tt(off[:, (b + 1) * 8:(b + 2) * 8], off[:, b * 8:(b + 1) * 8], tot[:, b * 8:(b + 1) * 8], ALU.add,
                   ("off", "tot"), ("off",))
            tt(G, ps[7][:, 0:NB * 8], off[:, 0:NB * 8], ALU.add, (("ps", 7), "off"), ("G",))
            G3 = G.rearrange("p (b h) -> p b h", h=NH)
            for c in range(NCH):
                nkb = 4 * (c + 1)
                for h in range(NH):
                    col = (4 * c + 2) * 8 + h
                    ts(biasT[c][:, h, 0:nkb], G3[:, 0:nkb, h], off[:, col:col + 1], ALU.subtract, ("G", "off"), ("biasT",))
            load_qk_w(0, 1536, 2048)
            for g in range(4):
                if g + 1 < 4:
                    load_qk_w(g + 1, 1536, 2048)
                proj_qk_fx(g)
                attn_fx_pair(g)
            A.release(MIX_TOP)
            Sx.barrier()

            wgate = A.bf(KC * 2048).rearrange("p (k n) -> p k n", k=KC)
            wosb = A.bf(4 * D).rearrange("p (h n) -> p h n", h=4)
            wofx = A.bf(4 * D).rearrange("p (h n) -> p h n", h=4)
            wout = A.bf(KC * D).rearrange("p (k n) -> p k n", k=KC)
            merged = A.bf(KC * CH).rearrange("p (k n) -> p k n", k=KC)
            xb3 = A.f32(KC * CH).rearrange("p (k n) -> p k n", k=KC)
            ysbc = A.bf(4 * CH).rearrange("p (h n) -> p h n", h=4)
            yfxc = A.bf(4 * CH).rearrange("p (h n) -> p h n", h=4)
            gs = A.f32(CH)
            gf = A.f32(CH)
            m1 = A.f32(CH)
            m2 = A.f32(CH)
            stg_alloc(KC * 128, 3)
            def ld_gate(q):
                wload(wgate[:, :, q * 128:(q + 1) * 128],
                      w_in_l[:, 3080 + q * 128:3080 + (q + 1) * 128].rearrange("(k p) n -> p k n", p=128), ("wgate", q), KC, 128)

            def ld_o(q):
                wload(wosb[:, :, q * 256:(q + 1) * 256],
                      w_o_sb[l][:, q * 256:(q + 1) * 256].rearrange("(h p) n -> p h n", p=128), ("wosb", q), 4, 256)
                wload(wofx[:, :, q * 256:(q + 1) * 256],
                      w_o_fox[l][:, q * 256:(q + 1) * 256].rearrange("(h p) n -> p h n", p=128), ("wofx", q), 4, 256)

            for j_ in range(KC):
                ld_gate(j_)
                ld_gate(8 + j_)
                if j_ % 2 == 0:
                    ld_o(j_ // 2)
            for q in range(8):
                wload(wout[:, :, q * 128:(q + 1) * 128],
                      w_out[l][:, q * 128:(q + 1) * 128].rearrange("(k p) n -> p k n", p=128), ("wout", q), KC, 128)
            for c in range(NCH):
                dma("sp", ysbc, ysb_scr[:, :, c * CH:(c + 1) * CH].rearrange("h d n -> d h n"), (), ("ysbc",))
                dma("sp", yfxc, yfx_scr[:, :, c * CH:(c + 1) * CH].rearrange("h d n -> d h n"), (), ("yfxc",))
                dma("sp", xb3, chunk_src(x_src, c), (), ("xb3",))
                for j in range(KC):
                    for kc in range(KC):
                        mm(ps[0], wgate[:, kc, j * 128:(j + 1) * 128], hT[:, kc, c * CH:(c + 1) * CH], kc == 0, kc == KC - 1,
                           (("wgate", j), ("hT", c)), (("ps", 0),))
                    act(gs, ps[0], AF.Sigmoid, (("ps", 0), "prm"), ("gs",), bias=prm_l[:, 16 + j:17 + j])
                    for kc in range(KC):
                        mm(ps[1], wgate[:, kc, 1024 + j * 128:1024 + (j + 1) * 128], hT[:, kc, c * CH:(c + 1) * CH],
                           kc == 0, kc == KC - 1, (("wgate", 8 + j), ("hT", c)), (("ps", 1),))
                    act(gf, ps[1], AF.Sigmoid, (("ps", 1), "prm"), ("gf",), bias=prm_l[:, 24 + j:25 + j])
                    for h in range(4):
                        mm(ps[2], wosb[:, h, j * 128:(j + 1) * 128], ysbc[:, h, :], h == 0, h == 3,
                           (("wosb", j // 2), "ysbc"), (("ps", 2),))
                    tt(m1, ps[2], gs, ALU.mult, (("ps", 2), "gs"), ("m1",))
                    for h in range(4):
                        mm(ps[3], wofx[:, h, j * 128:(j + 1) * 128], yfxc[:, h, :], h == 0, h == 3,
                           (("wofx", j // 2), "yfxc"), (("ps", 3),))
                    tt(m2, ps[3], gf, ALU.mult, (("ps", 3), "gf"), ("m2",))
                    tt(merged[:, j, :], m1, m2, ALU.add, ("m1", "m2"), (("mg", j),))
                for d in range(KC):
                    pb = 4 + d % 2
                    for j in range(KC):
                        mm(ps[pb], wout[:, j, d * 128:(d + 1) * 128], merged[:, j, :], j == 0, j == KC - 1,
                           (("wout", d), ("mg", j)), (("ps", pb),))
                    tt(xb3[:, d, :], ps[pb], xb3[:, d, :], ALU.add, (("ps", pb), "xb3"), ("xb3",))
                dma("sp", chunk_src(x1T, c), xb3, ("xb3",), ())
            A.release(CONST_TOP)
            Sx.barrier()

            moe = (l % 2 == 1)
            if not moe:
                n_exp = 2

                def gcols(e, fj):
                    return w_gu_dense[0][:, e * FE + fj * 128:e * FE + (fj + 1) * 128]

                def ucols(e, fj):
                    return w_gu_dense[0][:, DFF + e * FE + fj * 128:DFF + e * FE + (fj + 1) * 128]

                def dnrows(e, d):
                    return w_dn_dense[0][e * FE:(e + 1) * FE, d * 128:(d + 1) * 128]
            else:
                n_exp = NE

                def gcols(e, fj):
                    return w_gu_exp[0][e][:, fj * 128:(fj + 1) * 128]

                def ucols(e, fj):
                    return w_gu_exp[0][e][:, FE + fj * 128:FE + (fj + 1) * 128]

                def dnrows(e, d):
                    return w_dn_exp[0][e][:, d * 128:(d + 1) * 128]

            h2T = A.bf(KC * SC).rearrange("p (k n) -> p k n", k=KC)
            acc = A.f32(KC * SC).rearrange("p (k n) -> p k n", k=KC)
            aT_w = A.f32(FJ * SC // 2)
            aT = aT_w.bitcast(BF16).rearrange("p (f n) -> p f n", f=FJ)
            cbc = A.f32(SC)
            sg = [A.f32(CH) for _ in range(2)]
            a32 = [A.f32(CH) for _ in range(2)]
            wg = [A.bf(KC * 128).rearrange("p (k n) -> p k n", k=KC) for _ in range(2)]
            wu = [A.bf(KC * 128).rearrange("p (k n) -> p k n", k=KC) for _ in range(2)]
            wdn = [A.bf(FJ * 128).rearrange("p (f n) -> p f n", f=FJ) for _ in range(2)]
            tagp = {"lnv": A.f32(CH), "rstd": A.f32(CH)}
            assert FJ * SC // 2 >= KC * CH // 2 + KC * CH
            sq = aT_w[:, 0:KC * CH // 2].bitcast(BF16).rearrange("p (k n) -> p k n", k=KC)
            h2f = aT_w[:, KC * CH // 2:KC * CH // 2 + KC * CH].rearrange("p (k n) -> p k n", k=KC)
            wr = A.f32(KC * NE).rearrange("p (k n) -> p k n", k=KC)
            lg = A.f32(BPS * NE)
            comb = A.f32(BPS * NE)
            t1 = A.f32(NE)
            t2 = A.f32(NE)
            mx1 = A.f32(1)
            mx2 = A.f32(1)
            nmx1 = A.f32(1)
            den = A.f32(1)
            rden = A.f32(1)
            diag = [A.f32(128) for _ in range(2)]
            stg_alloc(FJ * 128, 3)
            if moe:
                dma("sp", wr, w_router[0].rearrange("(k p) n -> p k n", p=128), (), ("wr",))
            wcnt = {"gu": 0, "dn": 0, "G": 0, "D": 0, "s": 0, "dg": 0}
            for sc in range(NSC):
                for cc in range(CPS):
                    c = sc * CPS + cc
                    dma("sp", acc[:, :, cc * CH:(cc + 1) * CH], chunk_src(x1T, c), (), (("acc", cc),))
                for cc in range(CPS):
                    rms_chunk(acc[:, :, cc * CH:(cc + 1) * CH], ("acc", cc), sq, 8, prm_l,
                              lambda kc, cc=cc: (h2T[:, kc, cc * CH:(cc + 1) * CH], ("h2T", cc)), tagp,
                              want_f32=(lambda kc: (h2f[:, kc, :], "h2f")) if moe else None)
                    if moe:
                        for bb in range(4):
                            b = cc * 4 + bb
                            for kc in range(KC):
                                mm(ps[6][:, 0:NE], h2f[:, kc, bb * 128:(bb + 1) * 128], wr[:, kc, :], kc == 0, kc == KC - 1,
                                   ("h2f", "wr"), (("ps", 6),))
                            lgb = lg[:, b * NE:(b + 1) * NE]
                            cb = comb[:, b * NE:(b + 1) * NE]
                            cp(lgb, ps[6][:, 0:NE], (("ps", 6),), ("lg",))
                            add("dve", lambda h_, lgb=lgb: h_.tensor_reduce(out=mx1, in_=lgb, axis=AX.X, op=ALU.max), ("lg",), ("mx1",))
                            ts(t1, lgb, mx1, ALU.is_equal, ("lg", "mx1"), ("t1",), s2=-1e30, op1=ALU.mult)
                            tt(t1, t1, lgb, ALU.add, ("t1", "lg"), ("t1",))
                            add("dve", lambda h_: h_.tensor_reduce(out=mx2, in_=t1, axis=AX.X, op=ALU.max), ("t1",), ("mx2",))
                            ts(t2, lgb, mx2, ALU.is_ge, ("lg", "mx2"), ("t2",))
                            ts(nmx1, mx1, -1.0, ALU.mult, ("mx1",), ("nmx1",))
                            act(t1, lgb, AF.Exp, ("lg", "nmx1"), ("t1",), bias=nmx1)
                            tt(t1, t1, t2, ALU.mult, ("t1", "t2"), ("t1",))
                            add("dve", lambda h_: h_.tensor_reduce(out=den, in_=t1, axis=AX.X, op=ALU.add), ("t1",), ("den",))
                            add("dve", lambda h_: h_.reciprocal(out=rden, in_=den), ("den",), ("rden",))
                            ts(cb, t1, rden, ALU.mult, ("t1", "rden"), ("comb",))
                Sx.barrier()
                for e in range(n_exp):
                    if moe:
                        for cc in range(CPS):
                            for bb in range(4):
                                b = cc * 4 + bb
                                dg = diag[wcnt["dg"] % 2]
                                dk = ("diag", wcnt["dg"] % 2)
                                wcnt["dg"] += 1
                                ts(dg, ident_f, comb[:, b * NE + e:b * NE + e + 1], ALU.mult, ("comb", "c_ident"), (dk,))
                                mm(ps[7][:, bb * 128:(bb + 1) * 128], ones_f, dg, True, True, (dk, "c_onesf"), (("ps", 7),))
                            cp(cbc[:, cc * CH:(cc + 1) * CH], ps[7], (("ps", 7),), (("cbc", cc),))
                    for fj in range(FJ):
                        wp = wcnt["gu"] % 2
                        wcnt["gu"] += 1
                        wload(wg[wp], gcols(e, fj).rearrange("(k p) n -> p k n", p=128), ("wg", wp), KC, 128)
                        wload(wu[wp], ucols(e, fj).rearrange("(k p) n -> p k n", p=128), ("wu", wp), KC, 128)
                        for cc in range(CPS):
                            gb = wcnt["G"] % 2
                            wcnt["G"] += 1
                            sp_ = wcnt["s"] % 2
                            wcnt["s"] += 1
                            for kc in range(KC):
                                mm(ps[gb], wg[wp][:, kc, :], h2T[:, kc, cc * CH:(cc + 1) * CH], kc == 0, kc == KC - 1,
                                   (("wg", wp), ("h2T", cc)), (("ps", gb),))
                            for kc in range(KC):
                                mm(ps[2 + gb], wu[wp][:, kc, :], h2T[:, kc, cc * CH:(cc + 1) * CH], kc == 0, kc == KC - 1,
                                   (("wu", wp), ("h2T", cc)), (("ps", 2 + gb),))
                            act(sg[sp_], ps[gb], AF.Silu, (("ps", gb),), (("sg", sp_),))
                            if moe:
                                tt(a32[sp_], sg[sp_], ps[2 + gb], ALU.mult, (("sg", sp_), ("ps", 2 + gb)), (("a32", sp_),))
                                tt(aT[:, fj, cc * CH:(cc + 1) * CH], a32[sp_], cbc[:, cc * CH:(cc + 1) * CH], ALU.mult,
                                   (("a32", sp_), ("cbc", cc)), (("aT", fj, cc),))
                            else:
                                tt(aT[:, fj, cc * CH:(cc + 1) * CH], sg[sp_], ps[2 + gb], ALU.mult,
                                   (("sg", sp_), ("ps", 2 + gb)), (("aT", fj, cc),))
                    for d in range(KC):
                        wp = wcnt["dn"] % 2
                        wcnt["dn"] += 1
                        wload(wdn[wp], dnrows(e, d).rearrange("(f p) n -> p f n", p=128), ("wdn", wp), FJ, 128)
                        for cc in range(CPS):
                            db = 4 + wcnt["D"] % 2
                            wcnt["D"] += 1
                            for fj in range(FJ):
                                mm(ps[db], wdn[wp][:, fj, :], aT[:, fj, cc * CH:(cc + 1) * CH], fj == 0, fj == FJ - 1,
                                   (("wdn", wp), ("aT", fj, cc)), (("ps", db),))
                            tt(acc[:, d, cc * CH:(cc + 1) * CH], ps[db], acc[:, d, cc * CH:(cc + 1) * CH], ALU.add,
                               (("ps", db), ("acc", cc)), (("acc", cc),))
                for cc in range(CPS):
                    c = sc * CPS + cc
                    dma("sp", chunk_src(x_dst, c), acc[:, :, cc * CH:(cc + 1) * CH], (("acc", cc),), ())
                Sx.barrier()
            A.release(CONST_TOP)
        Sx.finish()
        Sx.emit()
    return nc


_NC_CACHE = {}


def _prep_inputs(inputs, S):
    x = np.asarray(inputs["x"], dtype=np.float32)
    B = x.shape[0]
    g_mix = np.asarray(inputs["g_mix"], np.float32)
    g_ffn = np.asarray(inputs["g_ffn"], np.float32)
    b_gate = np.asarray(inputs["b_gate"], np.float32)
    g_q = np.asarray(inputs["g_q"], np.float32)
    g_k = np.asarray(inputs["g_k"], np.float32)
    b_f = np.asarray(inputs["b_f"], np.float32)
    prm = np.zeros((2, 128, 64), np.float32)
    for l in range(2):
        prm[l, :, 0:8] = g_mix[l].reshape(8, 128).T
        prm[l, :, 8:16] = g_ffn[l].reshape(8, 128).T
        prm[l, :, 16:32] = b_gate[l].reshape(16, 128).T
        prm[l, :, 32] = np.tile(g_q[l], 2)
        prm[l, :, 33] = np.tile(g_k[l], 2)
        prm[l, :, 34:42] = np.broadcast_to(b_f[l][None, :], (128, 8))
    shared = {"prm": prm}
    for k in ("w_in", "w_o_sb", "w_o_fox", "w_out", "w_gu_dense", "w_dn_dense", "w_router", "w_gu_exp", "w_dn_exp"):
        shared[k] = np.ascontiguousarray(np.asarray(inputs[k], np.float32))
    in_maps = []
    for b in range(B):
        m = dict(shared)
        m["xT"] = np.ascontiguousarray(x[b].T)
        in_maps.append(m)
    return in_maps


def kernel(**inputs):
    x = inputs["x"]
    B, S, _ = x.shape
    key = (S,)
    if key not in _NC_CACHE:
        _NC_CACHE[key] = build_nc(S)
    nc = _NC_CACHE[key]
    in_maps = _prep_inputs(inputs, S)
    res = run_bass_kernel_spmd(nc, in_maps, core_ids=list(range(B)))
    out = np.stack([np.ascontiguousarray(res.results[b]["outT"].T) for b in range(B)], axis=0)
    return out.astype(np.float32)
```
